# Optimizing a Trainium2 kernel written in Bass

```python
import jax
import jax.numpy as jnp
from jax import lax
import numpy as np


D_MODEL = 4096
BATCH = 2
SEQ = 4096
DEPTH = 2

CTX_LEN = 256
GRID_W = 64
HEAD_DIM = 128
Q_BLOCK = 128
RET_CHUNK = 128
ROPE_BASE = 10000.0
NORM_EPS = 1e-6

MLA_HEADS = 8
MLA_Q_LORA = 1024
MLA_KV_LORA = 512
MLA_NOPE = 128
MLA_ROPE = 64
MLA_QK = MLA_NOPE + MLA_ROPE
MLA_V = 128
MLA_SCALE = MLA_QK ** -0.5
GQA_HEADS = 12
GQA_KV_HEADS = 4
GQA_SCALE = HEAD_DIM ** -0.5
RET_HEADS = 12
RET_DK = 128
RET_DV = 128
D_MIX = MLA_HEADS * MLA_V + GQA_HEADS * HEAD_DIM + RET_HEADS * RET_DV
IN_SPLITS = (MLA_Q_LORA, MLA_KV_LORA, MLA_ROPE,
             GQA_HEADS * HEAD_DIM, GQA_KV_HEADS * HEAD_DIM, GQA_KV_HEADS * HEAD_DIM,
             RET_HEADS * RET_DK, RET_HEADS * RET_DK, RET_HEADS * RET_DV,
             RET_HEADS * RET_DV, RET_HEADS * RET_DV)
D_IN = sum(IN_SPLITS)
N_EXPERTS = 16
N_GROUPS = 4
EXPERTS_PER_GROUP = N_EXPERTS // N_GROUPS
TOP_K = 2
D_EXPERT = 1024
D_SHARED = 1024
ROUTED_SCALE = 1.0

kernel_name = 'hybrid_mla_gqa_retention_moe_dit'


def rms_norm(x, g):
    xf = x.astype(jnp.float32)
    y = xf * lax.rsqrt(jnp.mean(xf * xf, axis=-1, keepdims=True) + NORM_EPS)
    return (y * g.astype(jnp.float32)).astype(x.dtype)


def modulate(h, shift, scale):
    return h * (1 + scale) + shift


def rope_freqs(dim):
    return ROPE_BASE ** (-jnp.arange(0, dim, 2, dtype=jnp.float32) / dim)


def axial_angles(n_rows, rot_dim):
    rows = jnp.repeat(jnp.arange(n_rows, dtype=jnp.float32), GRID_W)
    cols = jnp.tile(jnp.arange(GRID_W, dtype=jnp.float32), n_rows)
    f = rope_freqs(rot_dim // 2)
    return jnp.concatenate([rows[:, None] * f, cols[:, None] * f], axis=-1)


def apply_rope(x, angles):
    half = x.shape[-1] // 2
    cos = jnp.cos(angles)[:, None, :]
    sin = jnp.sin(angles)[:, None, :]
    xf = x.astype(jnp.float32)
    x1, x2 = xf[..., :half], xf[..., half:]
    return jnp.concatenate([x1 * cos - x2 * sin, x1 * sin + x2 * cos], axis=-1).astype(x.dtype)


def attend(q, k, v, scale):
    B, Lq, H, dq = q.shape
    Hkv = k.shape[2]
    G = H // Hkv
    nb = Lq // Q_BLOCK
    qb = q.reshape(B, nb, Q_BLOCK, Hkv, G, dq).transpose(1, 0, 2, 3, 4, 5)

    def one_block(q_blk):
        s = jnp.einsum('bqhgd,bkhd->bhgqk', q_blk, k, preferred_element_type=jnp.float32) * scale
        pr = jax.nn.softmax(s, axis=-1)
        return jnp.einsum('bhgqk,bkhd->bqhgd', pr.astype(v.dtype), v)

    o = lax.map(one_block, qb)
    return o.transpose(1, 0, 2, 3, 4, 5).reshape(B, Lq, H, v.shape[-1])


def mla_qkv(cq, ckv, kpe, p, angles):
    B, L, _ = cq.shape
    q = (rms_norm(cq, p['mla_q_lora_g']) @ p['w_uq']).reshape(B, L, MLA_HEADS, MLA_QK)
    kv = (rms_norm(ckv, p['mla_kv_lora_g']) @ p['w_ukv']).reshape(B, L, MLA_HEADS, MLA_NOPE + MLA_V)
    k = jnp.concatenate([kv[..., :MLA_NOPE],
                         jnp.broadcast_to(kpe[:, :, None, :], (B, L, MLA_HEADS, MLA_ROPE))], axis=-1)
    v = kv[..., MLA_NOPE:]
    q = rms_norm(q, p['mla_q_g'])
    k = rms_norm(k, p['mla_k_g'])
    if angles is not None:
        q = jnp.concatenate([q[..., :MLA_NOPE], apply_rope(q[..., MLA_NOPE:], angles)], axis=-1)
        k = jnp.concatenate([k[..., :MLA_NOPE], apply_rope(k[..., MLA_NOPE:], angles)], axis=-1)
    return q, k, v


def gqa_qkv(q, k, v, p, angles):
    B, L, _ = q.shape
    q = rms_norm(q.reshape(B, L, GQA_HEADS, HEAD_DIM), p['gqa_q_g'])
    k = rms_norm(k.reshape(B, L, GQA_KV_HEADS, HEAD_DIM), p['gqa_k_g'])
    v = v.reshape(B, L, GQA_KV_HEADS, HEAD_DIM)
    if angles is not None:
        q = apply_rope(q, angles)
        k = apply_rope(k, angles)
    return q, k, v


def retention_chunked(q, k, v, log_gamma, state0):
    B, L, H, _ = q.shape
    n = L // RET_CHUNK

    def to_chunks(a):
        return a.reshape(B, n, RET_CHUNK, H, a.shape[-1]).transpose(1, 0, 3, 2, 4)

    qc, kc, vc = to_chunks(q), to_chunks(k), to_chunks(v)
    idx = jnp.arange(RET_CHUNK, dtype=jnp.float32)
    diff = idx[:, None] - idx[None, :]
    intra = jnp.where(diff >= 0, jnp.exp(log_gamma[:, None, None] * jnp.maximum(diff, 0.0)), 0.0)
    q_decay = jnp.exp(log_gamma[:, None] * (idx + 1.0))
    k_decay = jnp.exp(log_gamma[:, None] * (RET_CHUNK - 1.0 - idx))
    chunk_decay = jnp.exp(log_gamma * RET_CHUNK)

    def step(state, inp):
        qb, kb, vb = inp
        s = jnp.einsum('bhid,bhjd->bhij', qb, kb) * intra[None]
        o = (jnp.einsum('bhij,bhjv->bhiv', s, vb)
             + jnp.einsum('bhid,bhdv->bhiv', qb, state) * q_decay[None, :, :, None])
        state = (state * chunk_decay[None, :, None, None]
                 + jnp.einsum('bhjd,bhjv->bhdv', kb * k_decay[None, :, :, None], vb))
        return state, o

    state, o = lax.scan(step, state0, (qc, kc, vc))
    o = o.transpose(1, 0, 3, 2, 4).reshape(B, L, H, v.shape[-1])
    return o, state


def head_group_norm(o):
    mu = jnp.mean(o, axis=-1, keepdims=True)
    var = jnp.mean(jnp.square(o - mu), axis=-1, keepdims=True)
    return (o - mu) * lax.rsqrt(var + NORM_EPS)


def retention_bidir(parts_c, parts_l, ang_c, ang_l, decay_f, decay_b, need_ctx):
    def heads(a, d):
        return a.reshape(a.shape[0], a.shape[1], RET_HEADS, d)

    def qkv(parts, ang):
        q = apply_rope(heads(parts[0], RET_DK), ang).astype(jnp.float32)
        k = apply_rope(heads(parts[1], RET_DK), ang).astype(jnp.float32) * (RET_DK ** -0.5)
        v = heads(parts[2], RET_DV).astype(jnp.float32)
        return q, k, v

    lg_f = -jnp.exp(decay_f.astype(jnp.float32))
    lg_b = -jnp.exp(decay_b.astype(jnp.float32))
    qc, kc, vc = qkv(parts_c, ang_c)
    ql, kl, vl = qkv(parts_l, ang_l)
    zero = jnp.zeros((qc.shape[0], RET_HEADS, RET_DK, RET_DV), jnp.float32)
    flip = lambda a: jnp.flip(a, axis=1)
    oc_f, s_f = retention_chunked(qc, kc, vc, lg_f, zero)
    ol_f, _ = retention_chunked(ql, kl, vl, lg_f, s_f)
    oc_b, s_b = retention_chunked(flip(qc), flip(kc), flip(vc), lg_b, zero)
    ol_b, _ = retention_chunked(flip(ql), flip(kl), flip(vl), lg_b, s_b)

    def merge(of, ob, parts):
        gf = heads(parts[3], RET_DV).astype(jnp.float32)
        gb = heads(parts[4], RET_DV).astype(jnp.float32)
        y = jax.nn.silu(gf) * head_group_norm(of) + jax.nn.silu(gb) * head_group_norm(ob)
        return y.reshape(y.shape[0], y.shape[1], RET_HEADS * RET_DV).astype(parts[0].dtype)

    out_l = merge(ol_f, flip(ol_b), parts_l)
    out_c = merge(oc_f, flip(oc_b), parts_c) if need_ctx else None
    return out_c, out_l


def moe_ffn(h, w_router, b_router, e_gate, e_up, e_down, s_gate, s_up, s_down):
    T = h.shape[0]
    scores = jax.nn.sigmoid(jnp.dot(h, w_router, preferred_element_type=jnp.float32))
    sel = scores + b_router.astype(jnp.float32)
    group_score = jnp.sum(lax.top_k(sel.reshape(T, N_GROUPS, EXPERTS_PER_GROUP), 2)[0], axis=-1)
    best_group = jnp.argmax(group_score, axis=-1)
    in_group = best_group[:, None] == (jnp.arange(N_EXPERTS) // EXPERTS_PER_GROUP)[None, :]
    _, top_idx = lax.top_k(jnp.where(in_group, sel, -jnp.inf), TOP_K)
    top_w = jnp.take_along_axis(scores, top_idx, axis=-1)
    top_w = top_w / jnp.sum(top_w, axis=-1, keepdims=True) * ROUTED_SCALE
    flat_e = top_idx.reshape(-1)
    order = jnp.argsort(flat_e)
    tok = order // TOP_K
    xs = h[tok]
    sizes = jnp.bincount(flat_e, length=N_EXPERTS).astype(jnp.int32)
    a = lax.ragged_dot(xs, e_gate, sizes)
    b = lax.ragged_dot(xs, e_up, sizes)
    y = lax.ragged_dot(jax.nn.silu(a) * b, e_down, sizes)
    y = (y.astype(jnp.float32) * top_w.reshape(-1)[order][:, None]).astype(h.dtype)
    routed = jax.ops.segment_sum(y, tok, num_segments=T)
    shared = (jax.nn.silu(h @ s_gate) * (h @ s_up)) @ s_down
    return routed + shared


def trunk_layer(x_c, x_l, c_act, c_ctx_act, ang_mla, ang_gqa, ang_ret_c, ang_ret_l, p,
                w_router, b_router, update_ctx):
    B, S, _ = x_l.shape
    sh_a, sc_a, g_a, sh_f, sc_f, g_f = [m[:, None, :] for m in
                                        jnp.split(c_act @ p['w_mod'] + p['b_mod'], 6, axis=-1)]
    csh_a, csc_a, cg_a, csh_f, csc_f, cg_f = jnp.split(c_ctx_act @ p['w_mod'] + p['b_mod'], 6, axis=-1)

    h = jnp.concatenate([modulate(rms_norm(x_c, p['g_mix']), csh_a, csc_a),
                         modulate(rms_norm(x_l, p['g_mix']), sh_a, sc_a)], axis=1)
    split_points = [int(i) for i in np.cumsum(IN_SPLITS)[:-1]]
    parts = jnp.split(h @ p['w_in'], split_points, axis=-1)
    pc = [a[:, :CTX_LEN] for a in parts]
    pl = [a[:, CTX_LEN:] for a in parts]

    mq_c, mk_c, mv_c = mla_qkv(pc[0], pc[1], pc[2], p, None)
    mq_l, mk_l, mv_l = mla_qkv(pl[0], pl[1], pl[2], p, ang_mla)
    mla_l = attend(mq_l, jnp.concatenate([mk_c, mk_l], axis=1),
                   jnp.concatenate([mv_c, mv_l], axis=1), MLA_SCALE)
    gq_c, gk_c, gv_c = gqa_qkv(pc[3], pc[4], pc[5], p, None)
    gq_l, gk_l, gv_l = gqa_qkv(pl[3], pl[4], pl[5], p, ang_gqa)
    gqa_l = attend(gq_l, jnp.concatenate([gk_c, gk_l], axis=1),
                   jnp.concatenate([gv_c, gv_l], axis=1), GQA_SCALE)
    ret_c, ret_l = retention_bidir(pc[6:], pl[6:], ang_ret_c, ang_ret_l,
                                   p['ret_decay_f'], p['ret_decay_b'], update_ctx)

    def merge_heads(mla, gqa, ret):
        b, n = mla.shape[0], mla.shape[1]
        return jnp.concatenate([mla.reshape(b, n, -1), gqa.reshape(b, n, -1), ret], axis=-1) @ p['w_out']

    x_l = x_l + g_a * merge_heads(mla_l, gqa_l, ret_l)
    if update_ctx:
        mla_c = attend(mq_c, mk_c, mv_c, MLA_SCALE)
        gqa_c = attend(gq_c, gk_c, gv_c, GQA_SCALE)
        x_c = x_c + cg_a * merge_heads(mla_c, gqa_c, ret_c)

    tok = modulate(rms_norm(x_l, p['g_ffn']), sh_f, sc_f)
    if update_ctx:
        tok = jnp.concatenate([modulate(rms_norm(x_c, p['g_ffn']), csh_f, csc_f), tok], axis=1)
    n_tok = tok.shape[1]
    f = moe_ffn(tok.reshape(-1, D_MODEL), w_router, b_router, p['e_gate'], p['e_up'], p['e_down'],
                p['s_gate'], p['s_up'], p['s_down']).reshape(B, n_tok, D_MODEL)
    x_l = x_l + g_f * f[:, n_tok - S:]
    if update_ctx:
        x_c = x_c + cg_f * f[:, :CTX_LEN]
    return x_c, x_l


def setup_inputs(seed: int = 0) -> dict:
    key = jax.random.key(seed)
    ks = jax.random.split(key, 32)
    f32 = jnp.float32
    L = DEPTH

    def normal(i, shape, scale):
        return jax.random.normal(ks[i], shape, f32) * scale

    def gain(i, shape):
        return 1.0 + 0.02 * jax.random.normal(ks[i], shape, f32)

    ret_base = jnp.log(-jnp.log1p(-(2.0 ** (-5.0 - jnp.arange(RET_HEADS, dtype=f32)))))
    return {
        'x': normal(0, (BATCH, SEQ, D_MODEL), 1.0),
        'c': normal(1, (BATCH, D_MODEL), 1.0),
        'ctx': normal(2, (BATCH, CTX_LEN, D_MODEL), 1.0),
        'c_ctx': normal(3, (D_MODEL,), 1.0),
        'g_mix': gain(4, (L, D_MODEL)),
        'g_ffn': gain(5, (L, D_MODEL)),
        'w_mod': normal(6, (L, D_MODEL, 6 * D_MODEL), 0.5 * D_MODEL ** -0.5),
        'b_mod': normal(7, (L, 6 * D_MODEL), 0.02),
        'w_in': normal(8, (L, D_MODEL, D_IN), D_MODEL ** -0.5),
        'mla_q_lora_g': gain(9, (L, MLA_Q_LORA)),
        'mla_kv_lora_g': gain(10, (L, MLA_KV_LORA)),
        'w_uq': normal(11, (L, MLA_Q_LORA, MLA_HEADS * MLA_QK), MLA_Q_LORA ** -0.5),
        'w_ukv': normal(12, (L, MLA_KV_LORA, MLA_HEADS * (MLA_NOPE + MLA_V)), MLA_KV_LORA ** -0.5),
        'mla_q_g': gain(13, (L, MLA_QK)),
        'mla_k_g': gain(14, (L, MLA_QK)),
        'gqa_q_g': gain(15, (L, HEAD_DIM)),
        'gqa_k_g': gain(16, (L, HEAD_DIM)),
        'ret_decay_f': ret_base[None, :] + normal(17, (L, RET_HEADS), 0.1),
        'ret_decay_b': ret_base[None, :] + normal(18, (L, RET_HEADS), 0.1),
        'w_out': normal(19, (L, D_MIX, D_MODEL), D_MIX ** -0.5),
        'w_router': normal(20, (D_MODEL, N_EXPERTS), D_MODEL ** -0.5),
        'b_router': normal(21, (N_EXPERTS,), 0.01),
        'e_gate': normal(22, (L, N_EXPERTS, D_MODEL, D_EXPERT), D_MODEL ** -0.5),
        'e_up': normal(23, (L, N_EXPERTS, D_MODEL, D_EXPERT), D_MODEL ** -0.5),
        'e_down': normal(24, (L, N_EXPERTS, D_EXPERT, D_MODEL), D_EXPERT ** -0.5),
        's_gate': normal(25, (L, D_MODEL, D_SHARED), D_MODEL ** -0.5),
        's_up': normal(26, (L, D_MODEL, D_SHARED), D_MODEL ** -0.5),
        's_down': normal(27, (L, D_SHARED, D_MODEL), D_SHARED ** -0.5),
    }


def reference(x, c, ctx, c_ctx, g_mix, g_ffn, w_mod, b_mod, w_in, mla_q_lora_g, mla_kv_lora_g,
              w_uq, w_ukv, mla_q_g, mla_k_g, gqa_q_g, gqa_k_g, ret_decay_f, ret_decay_b, w_out,
              w_router, b_router, e_gate, e_up, e_down, s_gate, s_up, s_down):
    seq = x.shape[1]
    n_rows = seq // GRID_W
    ang_mla = axial_angles(n_rows, MLA_ROPE)
    ang_gqa = axial_angles(n_rows, HEAD_DIM)
    ret_f = rope_freqs(RET_DK)
    ang_ret_c = jnp.arange(CTX_LEN, dtype=jnp.float32)[:, None] * ret_f
    ang_ret_l = (CTX_LEN + jnp.arange(seq, dtype=jnp.float32))[:, None] * ret_f
    c_act = jax.nn.silu(c)
    c_ctx_act = jax.nn.silu(c_ctx)
    x_c, x_l = ctx, x
    for l in range(DEPTH):
        p = {
            'g_mix': g_mix[l], 'g_ffn': g_ffn[l], 'w_mod': w_mod[l], 'b_mod': b_mod[l],
            'w_in': w_in[l], 'mla_q_lora_g': mla_q_lora_g[l], 'mla_kv_lora_g': mla_kv_lora_g[l],
            'w_uq': w_uq[l], 'w_ukv': w_ukv[l], 'mla_q_g': mla_q_g[l], 'mla_k_g': mla_k_g[l],
            'gqa_q_g': gqa_q_g[l], 'gqa_k_g': gqa_k_g[l],
            'ret_decay_f': ret_decay_f[l], 'ret_decay_b': ret_decay_b[l], 'w_out': w_out[l],
            'e_gate': e_gate[l], 'e_up': e_up[l], 'e_down': e_down[l],
            's_gate': s_gate[l], 's_up': s_up[l], 's_down': s_down[l],
        }
        x_c, x_l = trunk_layer(x_c, x_l, c_act, c_ctx_act, ang_mla, ang_gqa, ang_ret_c, ang_ret_l,
                               p, w_router, b_router, l < DEPTH - 1)
    return x_l
```

```python
from contextlib import ExitStack
import numpy as np
import ml_dtypes
import concourse.bass as bass
import concourse.mybir as mybir
from concourse.bass_utils import run_bass_kernel_spmd

F32 = mybir.dt.float32
BF16 = mybir.dt.bfloat16
I32 = mybir.dt.int32
AF = mybir.ActivationFunctionType
ALU = mybir.AluOpType
AX = mybir.AxisListType

NDMA = 24
SAME_ENGINE_SYNC = True
PIPE_DISPATCH = False
WSTEP = 8
D = 4096
NT = 10
TT = 1280
NTG = 34
ROWS = 4352
GROUPS = [(0, 10), (10, 18), (18, 26), (26, 34)]
DIN = 11840
EPS = 1e-6
QW = 8192
KVW = 7168
QO = dict(qm=0, qg=2048, qr=3584, gf=5120, gb=6656)
KO = dict(km=0, vm=2048, kg=3072, vg=3584, kr=4096, vr=5632)
CAP = 1280
NE = 16

W_CHUNKS = [(0, 512, 'cq', 0), (512, 512, 'cq', 1), (1024, 512, 'ckv', 0), (1536, 64, 'kpe', 0)]
_off = 1600
for _k, _n in [('gq', 3), ('gk', 1), ('gv', 1), ('rq', 3), ('rk', 3), ('rv', 3), ('gf', 3), ('gb', 3)]:
    for _i in range(_n):
        W_CHUNKS.append((_off, 512, _k, _i))
        _off += 512


class Prog:
    def __init__(self, nc, stack):
        self.nc = nc
        self.eng = {'pe': nc.tensor, 'act': nc.scalar, 'dve': nc.vector,
                    'pool': nc.gpsimd, 'sp': nc.sync}
        self.sem = {e: stack.enter_context(nc.semaphore("s_" + e)) for e in self.eng}
        self.cnt = {e: 0 for e in self.eng}
        self.seen = {e: {} for e in self.eng}
        self.dsem = [stack.enter_context(nc.semaphore("d%d" % i)) for i in range(NDMA)]
        self.dcnt = [0] * NDMA
        self.di = 0
        self.last_w = {}
        self.readers = {}
        self.n_ops = 0
        self.uid = 0

    def sb(self, st, name, shape, dt):
        self.uid += 1
        return st.enter_context(self.nc.sbuf_tensor("%s_%d" % (name, self.uid), list(shape), dt))

    def ps(self, st, name, shape, dt):
        return st.enter_context(self.nc.psum_tensor(name, list(shape), dt))

    def _deps(self, reads, writes):
        toks = []
        for k in reads:
            t = self.last_w.get(k)
            if t is not None:
                toks.append(t)
        for k in writes:
            t = self.last_w.get(k)
            if t is not None:
                toks.append(t)
            toks.extend(self.readers.get(k, ()))
        return toks

    def _emit_waits(self, e, toks):
        best = {}
        for (s, v) in toks:
            if v > best.get(id(s), (None, 0))[1]:
                best[id(s)] = (s, v)
        seen = self.seen[e]
        for sid, (s, v) in best.items():
            if s is self.sem[e] and (e == 'pe' or not SAME_ENGINE_SYNC):
                continue
            if seen.get(sid, 0) >= v:
                continue
            self.eng[e].wait_ge(s, v)
            seen[sid] = v

    def _commit(self, tok, reads, writes):
        for k in writes:
            self.last_w[k] = tok
            self.readers[k] = []
        for k in reads:
            self.readers.setdefault(k, []).append(tok)

    def op(self, e, fn, reads=(), writes=()):
        self._emit_waits(e, self._deps(reads, writes))
        ins = fn(self.eng[e])
        self.cnt[e] += 1
        ins.then_inc(self.sem[e], 1)
        tok = (self.sem[e], self.cnt[e])
        self._commit(tok, reads, writes)
        self.n_ops += 1
        return tok

    def dma(self, e, out, in_, reads=(), writes=(), **kw):
        k = self.di % NDMA
        self.di += 1
        toks = self._deps(reads, writes)
        if self.dcnt[k] > 0:
            toks.append((self.dsem[k], 16 * self.dcnt[k]))
        self._emit_waits(e, toks)
        ins = self.eng[e].dma_start(out=out, in_=in_, **kw)
        self.dcnt[k] += 1
        ins.then_inc(self.dsem[k], 16)
        tok = (self.dsem[k], 16 * self.dcnt[k])
        self._commit(tok, reads, writes)
        self.n_ops += 1
        return tok

    def barrier(self):
        toks = [(self.sem[e], self.cnt[e]) for e in self.eng if self.cnt[e] > 0]
        toks += [(self.dsem[k], 16 * self.dcnt[k]) for k in range(NDMA) if self.dcnt[k] > 0]
        for e in self.eng:
            self._emit_waits(e, toks)

    def finish(self):
        toks = [(self.sem[e], self.cnt[e]) for e in self.eng if self.cnt[e] > 0]
        toks += [(self.dsem[k], 16 * self.dcnt[k]) for k in range(NDMA) if self.dcnt[k] > 0]
        self._emit_waits('sp', toks)
        self.eng['sp'].nop()


class Ctx:
    pass


def rot(C, name, n):
    i = C.rotc.get(name, 0)
    C.rotc[name] = i + 1
    return i % n


def emit_consts(C):
    P, st = C.P, C.gst
    C.identf = P.sb(st, "identf", [128, 128], F32)
    C.ident = P.sb(st, "ident", [128, 128], BF16)
    C.ones = P.sb(st, "ones", [128, 128], BF16)
    C.neghalf = P.sb(st, "neghalf", [128, 16], F32)
    P.op('pool', lambda e: e.memset(C.identf[:], 1.0), writes=['identf'])
    P.op('pool', lambda e: e.affine_select(out=C.identf[:], in_=C.identf[:], pattern=[[-1, 128]],
                                           compare_op=ALU.is_equal, fill=0.0, base=0, channel_multiplier=1),
         reads=['identf'], writes=['identf'])
    P.op('dve', lambda e: e.tensor_copy(out=C.ident[:], in_=C.identf[:]), reads=['identf'], writes=['ident'])
    P.op('pool', lambda e: e.memset(C.ones[:], 1.0), writes=['ones'])
    P.op('pool', lambda e: e.memset(C.neghalf[:], -0.5), writes=['neghalf'])
    C.pb = [P.ps(st, "pb%d" % i, [128, 512], F32) for i in range(6)]
    C.pt = [P.ps(st, "pt%d" % i, [128, 1024], BF16) for i in range(2)]


def rstd_from_ss(C, ss_ap, n, dim, key, outkey):
    P = C.P
    P.op('dve', lambda e: e.tensor_scalar(out=ss_ap, in0=ss_ap, scalar1=1.0 / dim, scalar2=EPS, op0=ALU.mult, op1=ALU.add),
         reads=[key], writes=[key])
    P.op('pool', lambda e: e.tensor_tensor(out=ss_ap, in0=ss_ap, in1=C.neghalf[:, 0:n], op=ALU.pow),
         reads=[key, 'neghalf'], writes=[key])


def transpose_to(C, src_fn, nblk, dst_fn, rkeys, wkeys, dt=BF16):
    P = C.P
    grp = 8 if dt == BF16 else 4
    i = 0
    while i < nblk:
        g = min(grp, nblk - i)
        if dt == BF16:
            bi = rot(C, 'pt', 2)
            bank, bkey = C.pt[bi], 'pt%d' % bi
            ident = C.ident
        else:
            bi = 4 + rot(C, 'ptf', 2)
            bank, bkey = C.pb[bi], 'pb%d' % bi
            ident = C.identf
        for q in range(g):
            P.op('pe', lambda e: e.transpose(out=bank[:, q * 128:(q + 1) * 128], in_=src_fn(i + q), identity=ident[:]),
                 reads=rkeys + ['ident', 'identf'], writes=[bkey])
        dst_fn(i, g, bank[:, 0:g * 128], bkey)
        i += g


def copy_alt(C, out, in_, reads, writes):
    P = C.P
    if rot(C, 'cpalt', 2) == 0:
        P.op('act', lambda e: e.activation(out=out, in_=in_, func=AF.Copy), reads=reads, writes=writes)
    else:
        P.op('dve', lambda e: e.tensor_copy(out=out, in_=in_), reads=reads, writes=writes)


def bcast_load(C, dst, src_row, key, eng='sp'):
    C.P.dma(eng, dst, src_row.partition_broadcast(128), writes=[key])


def mod_bcast(C, dst, l, r, n0, n, key):
    C.P.dma('sp', dst[:, 0:n], C.d['modv'][l, r, n0:n0 + n].partition_broadcast(128), reads=['modv'], writes=[key])


def run_items(C, items):
    P = C.P

    def load(it):
        s = rot(C, 'wslot', 2)
        it['slot'] = s
        KC, nw, n0, W = it['KC'], it['nw'], it['n0'], it['W']
        step = WSTEP
        for k0 in range(0, KC, step):
            k1 = min(KC, k0 + step)
            P.dma('pool', C.wbuf[s][:, k0:k1, 0:nw],
                  W[k0 * 128:k1 * 128, n0:n0 + nw].rearrange("(kc p) n -> p kc n", p=128),
                  writes=['wb%d_%d' % (s, k0 // step)])

    n = len(items)
    for i in range(min(2, n)):
        load(items[i])
    for i, it in enumerate(items):
        if it.get('pre'):
            it['pre']()
        s = it['slot']
        for t in it['tiles']:
            bi = rot(C, 'psA', 2)
            ps, pskey = C.pb[bi], 'pb%d' % bi
            KC = it['KC']
            for kc in range(KC):
                P.op('pe', lambda e: e.matmul(ps[:, 0:it['nw']], lhsT=it['lhsT'](kc, t), rhs=C.wbuf[s][:, kc, 0:it['nw']],
                                              start=(kc == 0), stop=(kc == KC - 1)),
                     reads=it['rkeys'](t) + ['wb%d_%d' % (s, kc // WSTEP)], writes=[pskey])
            it['evac'](t, ps, pskey)
        if i + 2 < n:
            load(items[i + 2])
        if it.get('post'):
            it['post']()


def stage_mod(C):
    P, d = C.P, C.d
    with ExitStack() as st:
        cT = P.sb(st, "cT", [128, 32, 2], F32)
        bsb = [P.sb(st, "bsb%d" % i, [2, 512], F32) for i in range(2)]
        wf = [P.sb(st, "wf%d" % i, [128, 8, 512], F32) for i in range(4)]
        mo = [P.sb(st, "mo%d" % i, [2, 512], F32) for i in range(2)]
        P.dma('sp', cT[:], d['cT'][:, :, :], writes=['cT'])
        P.op('act', lambda e: e.activation(out=cT[:], in_=cT[:], func=AF.Silu), reads=['cT'], writes=['cT'])
        for l in range(2):
            for ci in range(48):
                n0 = ci * 512
                ps, pskey = C.pb[ci % 2], 'pb%d' % (ci % 2)
                for k0 in range(4):
                    wi = rot(C, 'wf', 4)
                    P.dma('sp', wf[wi][:], d['w_mod'][l, k0 * 1024:(k0 + 1) * 1024, n0:n0 + 512].rearrange("(kc p) n -> p kc n", p=128),
                          writes=['wf%d' % wi])
                    for kk in range(8):
                        kc = k0 * 8 + kk
                        P.op('pe', lambda e: e.matmul(ps[0:2, :], lhsT=cT[:, kc, :], rhs=wf[wi][:, kk, :], start=(kc == 0), stop=(kc == 31)),
                             reads=['cT', 'wf%d' % wi], writes=[pskey])
                mi = rot(C, 'mo', 2)
                P.dma('sp', bsb[mi][:], d['b_mod'][l, n0:n0 + 512].partition_broadcast(2), writes=['bsb%d' % mi])
                P.op('dve', lambda e: e.tensor_tensor(out=mo[mi][:], in0=ps[0:2, :], in1=bsb[mi][:], op=ALU.add),
                     reads=[pskey, 'bsb%d' % mi], writes=['mo%d' % mi])
                P.dma('sp', d['modv'][l, :, n0:n0 + 512], mo[mi][:], reads=['mo%d' % mi], writes=['modv'])
        P.barrier()


def stage_norm(C, l, xsrc, gname, cs_sh, cs_sc, tiles, tok_dst=None, router=None):
    P, d = C.P, C.d
    with ExitStack() as st:
        A = [P.sb(st, "A%d" % r, [128, D], F32) for r in range(2)]
        Bv = [P.sb(st, "B%d" % r, [128, D], F32) for r in range(2)]
        xt = [P.sb(st, "xt%d" % i, [128, D], F32) for i in range(2 if router is None else 1)]
        hb = [P.sb(st, "hb%d" % i, [128, D], BF16) for i in range(2 if router is None else 1)]
        ss = [P.sb(st, "ss%d" % i, [128, 1], F32) for i in range(2)]
        gm = xt[-1]
        gmk = 'xt%d' % (len(xt) - 1)
        bcast_load(C, gm[:], d[gname][l, :], gmk)
        rows = [0, 1] if any(t < 2 for t in tiles) else [0]
        for r in rows:
            mod_bcast(C, A[r][:], l, r, cs_sc * D, D, 'A%d' % r)
            mod_bcast(C, Bv[r][:], l, r, cs_sh * D, D, 'B%d' % r)
            P.op('dve', lambda e: e.scalar_tensor_tensor(out=A[r][:], in0=A[r][:], scalar=1.0, in1=gm[:], op0=ALU.add, op1=ALU.mult),
                 reads=['A%d' % r, gmk], writes=['A%d' % r])
        if router is not None:
            hl = [P.sb(st, "hl%d" % i, [128, D], BF16) for i in range(1)]
            hlT = [P.sb(st, "hlT%d" % i, [128, 32, 128], BF16) for i in range(1)]
            wrh = P.sb(st, "wrh", [128, 32, NE], BF16)
            wrl = P.sb(st, "wrl", [128, 32, NE], BF16)
            wr = xt[0][:, 0:32 * NE].rearrange("p (k n) -> p k n", n=NE)
            P.dma('sp', wr, d['w_router'].rearrange("(kc p) n -> p kc n", p=128), writes=['xt0'])
            P.op('dve', lambda e: e.tensor_copy(out=wrh[:], in_=wr), reads=['xt0'], writes=['wrh'])
            P.op('dve', lambda e: e.tensor_tensor(out=wrl[:], in0=wr, in1=wrh[:], op=ALU.subtract), reads=['xt0', 'wrh'], writes=['wrl'])
        for t in tiles:
            r = 1 if t < 2 else 0
            lt = t - C.g0
            xi = rot(C, 'xt', 2)
            x_, xk = xt[xi % len(xt)], 'xt%d' % (xi % len(xt))
            h_, hk = hb[xi % len(hb)], 'hb%d' % (xi % len(hb))
            s_, sk = ss[xi], 'ssn%d' % xi
            P.dma('sp', x_[:], xsrc[t * 128:(t + 1) * 128, :], reads=[('x', t)], writes=[xk])
            P.op('act', lambda e: e.activation(out=h_[:], in_=x_[:], func=AF.Square, accum_out=s_[:]), reads=[xk], writes=[hk, sk])
            rstd_from_ss(C, s_[:], 1, D, sk, sk)
            P.op('dve', lambda e: e.scalar_tensor_tensor(out=x_[:], in0=x_[:], scalar=s_[:, 0:1], in1=A[r][:], op0=ALU.mult, op1=ALU.mult),
                 reads=[xk, sk, 'A%d' % r], writes=[xk])
            if router is None:
                P.op('pool', lambda e: e.tensor_tensor(out=h_[:], in0=x_[:], in1=Bv[r][:], op=ALU.add),
                     reads=[xk, 'B%d' % r], writes=[hk])
            else:
                P.op('pool', lambda e: e.tensor_tensor(out=x_[:], in0=x_[:], in1=Bv[r][:], op=ALU.add),
                     reads=[xk, 'B%d' % r], writes=[xk])
                P.op('act', lambda e: e.activation(out=h_[:], in_=x_[:], func=AF.Copy), reads=[xk], writes=[hk])
            if tok_dst is not None:
                P.dma('sp', tok_dst[t * 128:(t + 1) * 128, :], h_[:], reads=[hk], writes=[('h2tok', t)])
            def dst(i, g, src, bkey):
                copy_alt(C, C.big3[:, i:i + g, lt * 128:(lt + 1) * 128], src.rearrange("p (a b) -> p a b", b=128),
                         [bkey], [('big', t)])
            transpose_to(C, lambda i: h_[:, i * 128:(i + 1) * 128], 32, dst, [hk], None)
            if router is not None:
                li = 0
                P.op('dve', lambda e: e.tensor_tensor(out=hl[li][:], in0=x_[:], in1=h_[:], op=ALU.subtract), reads=[xk, hk], writes=['hl%d' % li])

                def dstl(i, g, src, bkey):
                    copy_alt(C, hlT[li][:, i:i + g, :], src.rearrange("p (a b) -> p a b", b=128), [bkey], ['hlT%d' % li])
                transpose_to(C, lambda i: hl[li][:, i * 128:(i + 1) * 128], 32, dstl, ['hl%d' % li], None)
                psr, psrk = C.pb[3], 'pb3'
                n_mm = 0
                for kc in range(32):
                    for (lh, lkey, rw, rkey) in ((C.big3[:, kc, lt * 128:(lt + 1) * 128], ('big', t), wrh, 'wrh'),
                                                 (hlT[li][:, kc, :], 'hlT%d' % li, wrh, 'wrh'),
                                                 (C.big3[:, kc, lt * 128:(lt + 1) * 128], ('big', t), wrl, 'wrl')):
                        P.op('pe', lambda e: e.matmul(psr[:, 0:NE], lhsT=lh, rhs=rw[:, kc, :], start=(n_mm == 0), stop=(n_mm == 95)),
                             reads=[lkey, rkey], writes=[psrk])
                        n_mm += 1
                P.op('act', lambda e: e.activation(out=router['scores'][:, t, :], in_=psr[:, 0:NE], func=AF.Sigmoid),
                     reads=[psrk], writes=['scores'])
        P.barrier()


def stage_win(C, l, tiles):
    P, d = C.P, C.d
    with ExitStack() as st:
        C.wbuf = [P.sb(st, "wbuf%d" % i, [128, 32, 512], BF16) for i in range(2)]
        stgf = [P.sb(st, "stgf%d" % i, [128, 512], F32) for i in range(3)]
        stgb = [P.sb(st, "stgb%d" % i, [128, 512], BF16) for i in range(4)]
        xs = [P.sb(st, "xs%d" % i, [128, 512], F32) for i in range(3)]
        tm = [P.sb(st, "tm%d" % i, [128, 512], F32) for i in range(3)]
        ssq = [P.sb(st, "ssq%d" % i, [128, 4], F32) for i in range(3)]
        tr = [[P.sb(st, "tr%d_%d" % (i, q), [128, 256], F32) for q in range(4)] for i in range(2)]
        gq = P.sb(st, "gqg", [128, 128], F32)
        gk = P.sb(st, "gkg", [128, 128], F32)
        ropeg = P.sb(st, "ropeg", [128, NT, 128], F32)
        roper = P.sb(st, "roper", [128, NT, 128], F32)
        bcast_load(C, gq[:], d['gqa_q_g'][l, :], 'gqg')
        bcast_load(C, gk[:], d['gqa_k_g'][l, :], 'gkg')
        g0, ng = C.g0, len(tiles)
        P.dma('sp', ropeg[:, 0:ng, :], d['rope_gqa'][g0 * 128:(g0 + ng) * 128, :].rearrange("(t p) c -> p t c", p=128), writes=['ropeg'])
        P.dma('sp', roper[:, 0:ng, :], d['rope_ret'][g0 * 128:(g0 + ng) * 128, :].rearrange("(t p) c -> p t c", p=128), writes=['roper'])

        def evac(ci):
            n0, nw, kind, sub = W_CHUNKS[ci]

            def f(t, ps, pskey):
                rows = slice(t * 128, (t + 1) * 128)
                if kind in ('cq', 'ckv', 'kpe'):
                    si = rot(C, 'stgf', 3)
                    P.op('act', lambda e: e.activation(out=stgf[si][:, 0:nw], in_=ps[:, 0:nw], func=AF.Copy),
                         reads=[pskey], writes=['stgf%d' % si])
                    P.dma('sp', d['lr'][rows, n0:n0 + nw], stgf[si][:, 0:nw], reads=['stgf%d' % si], writes=[('lr', t)])
                    return
                si = rot(C, 'stgb', 4)
                sb_, sbk = stgb[si], 'stgb%d' % si
                if kind in ('gv', 'rv', 'gf', 'gb'):
                    fn = AF.Silu if kind in ('gf', 'gb') else AF.Copy
                    P.op('act', lambda e: e.activation(out=sb_[:], in_=ps[:], func=fn), reads=[pskey], writes=[sbk])
                    if kind == 'gv':
                        dst, dk = d['kv'][rows, KO['vg']:KO['vg'] + 512], ('kv', t)
                    elif kind == 'rv':
                        dst, dk = d['kv'][rows, KO['vr'] + sub * 512:KO['vr'] + (sub + 1) * 512], ('kv', t)
                    elif kind == 'gf':
                        dst, dk = d['qkv'][rows, QO['gf'] + sub * 512:QO['gf'] + (sub + 1) * 512], ('qkv', t)
                    else:
                        dst, dk = d['qkv'][rows, QO['gb'] + sub * 512:QO['gb'] + (sub + 1) * 512], ('qkv', t)
                    P.dma('sp', dst, sb_[:], reads=[sbk], writes=[dk])
                    return
                xi = rot(C, 'xs', 3)
                x_, xk = xs[xi], 'xs%d' % xi
                scale = (128.0 ** -0.5) if kind == 'rk' else 1.0
                P.op('act', lambda e: e.activation(out=x_[:], in_=ps[:], func=AF.Copy, scale=scale), reads=[pskey], writes=[xk])
                x3 = x_[:].rearrange("p (h c) -> p h c", c=128)
                if kind in ('gq', 'gk'):
                    t_, tk = tm[xi], 'tm%d' % xi
                    s_, sk = ssq[xi], 'ssq%d' % xi
                    P.op('dve', lambda e: e.tensor_tensor(out=t_[:], in0=x_[:], in1=x_[:], op=ALU.mult), reads=[xk], writes=[tk])
                    P.op('dve', lambda e: e.tensor_reduce(out=s_[:], in_=t_[:].rearrange("p (h c) -> p h c", c=128), axis=AX.X, op=ALU.add),
                         reads=[tk], writes=[sk])
                    rstd_from_ss(C, s_[:], 4, 128, sk, sk)
                    P.op('dve', lambda e: e.tensor_tensor(out=x3, in0=x3, in1=s_[:].unsqueeze(2).to_broadcast([128, 4, 128]), op=ALU.mult),
                         reads=[xk, sk], writes=[xk])
                    g_ = gq if kind == 'gq' else gk
                    P.op('dve', lambda e: e.tensor_tensor(out=x3, in0=x3, in1=g_[:].unsqueeze(1).to_broadcast([128, 4, 128]), op=ALU.mult),
                         reads=[xk, 'gqg', 'gkg'], writes=[xk])
                    rp = ropeg
                else:
                    rp = roper
                cosb = rp[:, t - C.g0, 0:64].unsqueeze(1).to_broadcast([128, 4, 64])
                sinb = rp[:, t - C.g0, 64:128].unsqueeze(1).to_broadcast([128, 4, 64])
                x1, x2 = x3[:, :, 0:64], x3[:, :, 64:128]
                o3 = sb_[:].rearrange("p (h c) -> p h c", c=128)
                ri = rot(C, 'tr', 2)
                tA, tB, tC, tD = [tr[ri][q][:].rearrange("p (h c) -> p h c", c=64) for q in range(4)]
                kA, kB, kC, kD = ['tr%d_%d' % (ri, q) for q in range(4)]
                P.op('dve', lambda e: e.tensor_tensor(out=tA, in0=x1, in1=cosb, op=ALU.mult), reads=[xk, 'ropeg', 'roper'], writes=[kA])
                P.op('pool', lambda e: e.tensor_tensor(out=tB, in0=x2, in1=sinb, op=ALU.mult), reads=[xk, 'ropeg', 'roper'], writes=[kB])
                P.op('pool', lambda e: e.tensor_tensor(out=tC, in0=x1, in1=sinb, op=ALU.mult), reads=[xk, 'ropeg', 'roper'], writes=[kC])
                P.op('dve', lambda e: e.tensor_tensor(out=tD, in0=x2, in1=cosb, op=ALU.mult), reads=[xk, 'ropeg', 'roper'], writes=[kD])
                P.op('dve', lambda e: e.tensor_tensor(out=o3[:, :, 0:64], in0=tA, in1=tB, op=ALU.subtract), reads=[kA, kB], writes=[sbk])
                P.op('pool', lambda e: e.tensor_tensor(out=o3[:, :, 64:128], in0=tC, in1=tD, op=ALU.add), reads=[kC, kD], writes=[sbk])
                if kind == 'gq':
                    dst, dk = d['qkv'][rows, QO['qg'] + sub * 512:QO['qg'] + (sub + 1) * 512], ('qkv', t)
                elif kind == 'gk':
                    dst, dk = d['kv'][rows, KO['kg']:KO['kg'] + 512], ('kv', t)
                elif kind == 'rq':
                    dst, dk = d['qkv'][rows, QO['qr'] + sub * 512:QO['qr'] + (sub + 1) * 512], ('qkv', t)
                else:
                    dst, dk = d['kv'][rows, KO['kr'] + sub * 512:KO['kr'] + (sub + 1) * 512], ('kv', t)
                P.dma('sp', dst, sb_[:], reads=[sbk], writes=[dk])
            return f

        items = []
        for ci, (n0, nw, kind, sub) in enumerate(W_CHUNKS):
            items.append(dict(W=d['w_in'][l], n0=n0, nw=nw, KC=32, lhsT=lambda kc, t: C.big3[:, kc, (t - C.g0) * 128:(t - C.g0 + 1) * 128],
                              tiles=list(tiles), rkeys=lambda t: [('big', t)], evac=evac(ci)))
        run_items(C, items)
        P.barrier()


def stage_mla(C, l, tiles):
    P, d = C.P, C.d
    with ExitStack() as st:
        wuq = P.sb(st, "wuq", [128, 8, 1536], BF16)
        wukv = P.sb(st, "wukv", [128, 4, 2048], BF16)
        gql = P.sb(st, "gql", [128, 1024], F32)
        gkvl = P.sb(st, "gkvl", [128, 512], F32)
        gmq = P.sb(st, "gmq", [128, 192], F32)
        gmk = P.sb(st, "gmk", [128, 192], F32)
        ropem = P.sb(st, "ropem", [128, NT, 64], F32)
        lrt = [P.sb(st, "lrt%d" % i, [128, 1600], F32) for i in range(2)]
        junk = P.sb(st, "junk", [128, 1024], F32)
        nb = [P.sb(st, "nb%d" % i, [128, 1024], BF16) for i in range(2)]
        cT = [P.sb(st, "cT%d" % i, [128, 8, 128], BF16) for i in range(2)]
        ss1 = [P.sb(st, "ss1_%d" % i, [128, 4], F32) for i in range(4)]
        spb = [P.sb(st, "spb%d" % i, [128, 1], F32) for i in range(2)]
        xs = [P.sb(st, "xsm%d" % i, [128, 512], F32) for i in range(3)]
        tm = [P.sb(st, "tmm%d" % i, [128, 512], F32) for i in range(3)]
        kpn = [P.sb(st, "kpn%d" % i, [128, 64], F32) for i in range(3)]
        tr = [[P.sb(st, "trm%d_%d" % (i, q), [128, 64], F32) for q in range(4)] for i in range(3)]
        og = [P.sb(st, "og%d" % i, [128, 512], BF16) for i in range(4)]
        ov = [P.sb(st, "ov%d" % i, [128, 256], BF16) for i in range(3)]
        P.dma('pool', wuq[:], d['w_uq'][l].rearrange("(kc p) n -> p kc n", p=128), writes=['wuq'])
        P.dma('pool', wukv[:], d['w_ukv'][l].rearrange("(kc p) n -> p kc n", p=128), writes=['wukv'])
        bcast_load(C, gql[:], d['mla_q_lora_g'][l, :], 'gql')
        bcast_load(C, gkvl[:], d['mla_kv_lora_g'][l, :], 'gkvl')
        bcast_load(C, gmq[:], d['mla_q_g'][l, :], 'gmq')
        bcast_load(C, gmk[:], d['mla_k_g'][l, :], 'gmk')
        g0, ng = C.g0, len(tiles)
        P.dma('sp', ropem[:, 0:ng, :], d['rope_mla'][g0 * 128:(g0 + ng) * 128, :].rearrange("(t p) c -> p t c", p=128), writes=['ropem'])
        for i in range(4):
            P.op('pool', lambda e: e.memset(og[i][:], 0.0), writes=['og%d' % i])

        def rope32(x1, x2, o1, o2, t, rkeys, wkey, nh):
            cosb = ropem[:, t - C.g0, 0:32].unsqueeze(1).to_broadcast([128, nh, 32])
            sinb = ropem[:, t - C.g0, 32:64].unsqueeze(1).to_broadcast([128, nh, 32])
            ri = rot(C, 'trm', 3)
            tA, tB, tC, tD = [tr[ri][q][:, 0:nh * 32].rearrange("p (h c) -> p h c", c=32) for q in range(4)]
            kA, kB, kC, kD = ['trm%d_%d' % (ri, q) for q in range(4)]
            P.op('dve', lambda e: e.tensor_tensor(out=tA, in0=x1, in1=cosb, op=ALU.mult), reads=rkeys + ['ropem'], writes=[kA])
            P.op('pool', lambda e: e.tensor_tensor(out=tB, in0=x2, in1=sinb, op=ALU.mult), reads=rkeys + ['ropem'], writes=[kB])
            P.op('pool', lambda e: e.tensor_tensor(out=tC, in0=x1, in1=sinb, op=ALU.mult), reads=rkeys + ['ropem'], writes=[kC])
            P.op('dve', lambda e: e.tensor_tensor(out=tD, in0=x2, in1=cosb, op=ALU.mult), reads=rkeys + ['ropem'], writes=[kD])
            P.op('dve', lambda e: e.tensor_tensor(out=o1, in0=tA, in1=tB, op=ALU.subtract), reads=[kA, kB], writes=[wkey])
            P.op('pool', lambda e: e.tensor_tensor(out=o2, in0=tC, in1=tD, op=ALU.add), reads=[kC, kD], writes=[wkey])

        for t in tiles:
            rows = slice(t * 128, (t + 1) * 128)
            li = rot(C, 'lrt', 2)
            lr_, lk = lrt[li], 'lrt%d' % li
            P.dma('sp', lr_[:], d['lr'][rows, :], reads=[('lr', t)], writes=[lk])
            si = rot(C, 'ss1', 4)
            s_, sk = ss1[si], 'ss1_%d' % si
            P.op('act', lambda e: e.activation(out=junk[:, 0:1024], in_=lr_[:, 0:1024], func=AF.Square, accum_out=s_[:, 0:1]),
                 reads=[lk], writes=['junk', sk])
            rstd_from_ss(C, s_[:, 0:1], 1, 1024, sk, sk)
            ni = rot(C, 'nb', 2)
            P.op('dve', lambda e: e.scalar_tensor_tensor(out=nb[ni][:, 0:1024], in0=lr_[:, 0:1024], scalar=s_[:, 0:1], in1=gql[:],
                                                         op0=ALU.mult, op1=ALU.mult), reads=[lk, sk, 'gql'], writes=['nb%d' % ni])
            ci_ = rot(C, 'cT', 2)

            def dstq(i, g, src, bkey):
                copy_alt(C, cT[ci_][:, i:i + g, :], src.rearrange("p (a b) -> p a b", b=128), [bkey], ['cT%d' % ci_])
            transpose_to(C, lambda i: nb[ni][:, i * 128:(i + 1) * 128], 8, dstq, ['nb%d' % ni], None)
            for c4 in range(4):
                bi = 2 + rot(C, 'psM', 2)
                ps, pskey = C.pb[bi], 'pb%d' % bi
                for kc in range(8):
                    P.op('pe', lambda e: e.matmul(ps[:, 0:384], lhsT=cT[ci_][:, kc, :], rhs=wuq[:, kc, c4 * 384:(c4 + 1) * 384],
                                                  start=(kc == 0), stop=(kc == 7)), reads=['cT%d' % ci_, 'wuq'], writes=[pskey])
                xi = rot(C, 'xsm', 3)
                x_, xk = xs[xi], 'xsm%d' % xi
                t_, tk = tm[xi], 'tmm%d' % xi
                P.op('act', lambda e: e.activation(out=x_[:, 0:384], in_=ps[:, 0:384], func=AF.Copy), reads=[pskey], writes=[xk])
                x3 = x_[:, 0:384].rearrange("p (h c) -> p h c", c=192)
                si = rot(C, 'ss1', 4)
                s_, sk = ss1[si], 'ss1_%d' % si
                P.op('dve', lambda e: e.tensor_tensor(out=t_[:, 0:384], in0=x_[:, 0:384], in1=x_[:, 0:384], op=ALU.mult), reads=[xk], writes=[tk])
                P.op('dve', lambda e: e.tensor_reduce(out=s_[:, 0:2], in_=t_[:, 0:384].rearrange("p (h c) -> p h c", c=192), axis=AX.X, op=ALU.add),
                     reads=[tk], writes=[sk])
                rstd_from_ss(C, s_[:, 0:2], 2, 192, sk, sk)
                P.op('dve', lambda e: e.tensor_tensor(out=x3, in0=x3, in1=s_[:, 0:2].unsqueeze(2).to_broadcast([128, 2, 192]), op=ALU.mult),
                     reads=[xk, sk], writes=[xk])
                P.op('dve', lambda e: e.tensor_tensor(out=x3, in0=x3, in1=gmq[:].unsqueeze(1).to_broadcast([128, 2, 192]), op=ALU.mult),
                     reads=[xk, 'gmq'], writes=[xk])
                oi = rot(C, 'og', 4)
                o3 = og[oi][:].rearrange("p (h c) -> p h c", c=256)
                ok = 'og%d' % oi
                P.op('act', lambda e: e.activation(out=o3[:, :, 0:128], in_=x3[:, :, 0:128], func=AF.Copy), reads=[xk], writes=[ok])
                rope32(x3[:, :, 128:160], x3[:, :, 160:192], o3[:, :, 128:160], o3[:, :, 160:192], t, [xk], ok, 2)
                P.dma('sp', d['qkv'][rows, QO['qm'] + c4 * 512:QO['qm'] + (c4 + 1) * 512], og[oi][:], reads=[ok], writes=[('qkv', t)])
            si = rot(C, 'ss1', 4)
            s_, sk = ss1[si], 'ss1_%d' % si
            P.op('act', lambda e: e.activation(out=junk[:, 0:512], in_=lr_[:, 1024:1536], func=AF.Square, accum_out=s_[:, 0:1]),
                 reads=[lk], writes=['junk', sk])
            rstd_from_ss(C, s_[:, 0:1], 1, 512, sk, sk)
            ni = rot(C, 'nb', 2)
            P.op('dve', lambda e: e.scalar_tensor_tensor(out=nb[ni][:, 0:512], in0=lr_[:, 1024:1536], scalar=s_[:, 0:1], in1=gkvl[:],
                                                         op0=ALU.mult, op1=ALU.mult), reads=[lk, sk, 'gkvl'], writes=['nb%d' % ni])
            ci_ = rot(C, 'cT', 2)

            def dstk(i, g, src, bkey):
                copy_alt(C, cT[ci_][:, i:i + g, :], src.rearrange("p (a b) -> p a b", b=128), [bkey], ['cT%d' % ci_])
            transpose_to(C, lambda i: nb[ni][:, i * 128:(i + 1) * 128], 4, dstk, ['nb%d' % ni], None)
            spi = rot(C, 'spb', 2)
            sp_, spk = spb[spi], 'spb%d' % spi
            P.op('act', lambda e: e.activation(out=junk[:, 0:64], in_=lr_[:, 1536:1600], func=AF.Square, accum_out=sp_[:, 0:1]),
                 reads=[lk], writes=['junk', spk])
            for c4 in range(4):
                bi = 2 + rot(C, 'psM', 2)
                ps, pskey = C.pb[bi], 'pb%d' % bi
                for kc in range(4):
                    P.op('pe', lambda e: e.matmul(ps[:, 0:512], lhsT=cT[ci_][:, kc, :], rhs=wukv[:, kc, c4 * 512:(c4 + 1) * 512],
                                                  start=(kc == 0), stop=(kc == 3)), reads=['cT%d' % ci_, 'wukv'], writes=[pskey])
                xi = rot(C, 'xsm', 3)
                x_, xk = xs[xi], 'xsm%d' % xi
                t_, tk = tm[xi], 'tmm%d' % xi
                P.op('act', lambda e: e.activation(out=x_[:], in_=ps[:], func=AF.Copy), reads=[pskey], writes=[xk])
                x4 = x_[:].rearrange("p (h s c) -> p h s c", s=2, c=128)
                vi = rot(C, 'ov', 3)
                P.op('act', lambda e: e.activation(out=ov[vi][:].rearrange("p (h c) -> p h c", c=128), in_=x4[:, :, 1, :], func=AF.Copy),
                     reads=[xk], writes=['ov%d' % vi])
                P.dma('sp', d['kv'][rows, KO['vm'] + c4 * 256:KO['vm'] + (c4 + 1) * 256], ov[vi][:], reads=['ov%d' % vi], writes=[('kv', t)])
                si = rot(C, 'ss1', 4)
                s_, sk = ss1[si], 'ss1_%d' % si
                t4 = t_[:].rearrange("p (h s c) -> p h s c", s=2, c=128)
                P.op('dve', lambda e: e.tensor_tensor(out=t4[:, :, 0, :], in0=x4[:, :, 0, :], in1=x4[:, :, 0, :], op=ALU.mult), reads=[xk], writes=[tk])
                P.op('dve', lambda e: e.tensor_reduce(out=s_[:, 0:2], in_=t4[:, :, 0, :], axis=AX.X, op=ALU.add), reads=[tk], writes=[sk])
                P.op('dve', lambda e: e.tensor_scalar(out=s_[:, 0:2], in0=s_[:, 0:2], scalar1=sp_[:, 0:1], scalar2=None, op0=ALU.add),
                     reads=[sk, spk], writes=[sk])
                rstd_from_ss(C, s_[:, 0:2], 2, 192, sk, sk)
                oi = rot(C, 'og', 4)
                o3 = og[oi][:].rearrange("p (h c) -> p h c", c=256)
                ok = 'og%d' % oi
                P.op('dve', lambda e: e.tensor_tensor(out=t4[:, :, 0, :], in0=x4[:, :, 0, :], in1=s_[:, 0:2].unsqueeze(2).to_broadcast([128, 2, 128]), op=ALU.mult),
                     reads=[xk, sk], writes=[tk])
                P.op('dve', lambda e: e.tensor_tensor(out=o3[:, :, 0:128], in0=t4[:, :, 0, :], in1=gmk[:, 0:128].unsqueeze(1).to_broadcast([128, 2, 128]), op=ALU.mult),
                     reads=[tk, 'gmk'], writes=[ok])
                ki = rot(C, 'kpn', 3)
                P.op('dve', lambda e: e.tensor_tensor(out=kpn[ki][:], in0=lr_[:, 1536:1600], in1=gmk[:, 128:192], op=ALU.mult),
                     reads=[lk, 'gmk'], writes=['kpn%d' % ki])
                kpt = tm[(xi + 1) % 3]
                kptk = 'tmm%d' % ((xi + 1) % 3)
                kp3 = kpt[:, 0:128].rearrange("p (h c) -> p h c", c=64)
                P.op('dve', lambda e: e.tensor_tensor(out=kp3, in0=kpn[ki][:].unsqueeze(1).to_broadcast([128, 2, 64]),
                                                      in1=s_[:, 0:2].unsqueeze(2).to_broadcast([128, 2, 64]), op=ALU.mult),
                     reads=['kpn%d' % ki, sk], writes=[kptk])
                rope32(kp3[:, :, 0:32], kp3[:, :, 32:64], o3[:, :, 128:160], o3[:, :, 160:192], t, [kptk], ok, 2)
                P.dma('sp', d['kv'][rows, KO['km'] + c4 * 512:KO['km'] + (c4 + 1) * 512], og[oi][:], reads=[ok], writes=[('kv', t)])
        P.barrier()


def stage_ret(C, l, upd_ctx, heads=range(12)):
    P, d = C.P, C.d
    with ExitStack() as st:
        ti = P.sb(st, "ti32", [128, 128], I32)
        dif = P.sb(st, "dif", [128, 128], F32)
        pos = [P.sb(st, "posd%d" % i, [128, 128], F32) for i in range(2)]
        msk = [P.sb(st, "mskd%d" % i, [128, 128], F32) for i in range(2)]
        rowv = [P.sb(st, "rowv%d" % i, [128, 128], F32) for i in range(2)]
        colv = P.sb(st, "colv", [128, 2], F32)
        lg = P.sb(st, "lg", [128, 2, 12], F32)
        cd = P.sb(st, "cd", [128, 2, 12], F32)
        kd = P.sb(st, "kd", [128, 2, 12], F32)
        P.op('pool', lambda e: e.iota(out=ti[:], pattern=[[1, 128]], base=0, channel_multiplier=-1), writes=['ti32'])
        P.op('dve', lambda e: e.tensor_copy(out=dif[:], in_=ti[:]), reads=['ti32'], writes=['dif'])
        P.op('dve', lambda e: e.tensor_scalar(out=pos[0][:], in0=dif[:], scalar1=0.0, scalar2=None, op0=ALU.max), reads=['dif'], writes=['posd0'])
        P.op('dve', lambda e: e.tensor_scalar(out=pos[1][:], in0=dif[:], scalar1=-1.0, scalar2=0.0, op0=ALU.mult, op1=ALU.max), reads=['dif'], writes=['posd1'])
        P.op('dve', lambda e: e.tensor_scalar(out=msk[0][:], in0=dif[:], scalar1=0.0, scalar2=None, op0=ALU.is_ge), reads=['dif'], writes=['mskd0'])
        P.op('dve', lambda e: e.tensor_scalar(out=msk[1][:], in0=dif[:], scalar1=0.0, scalar2=None, op0=ALU.is_le), reads=['dif'], writes=['mskd1'])
        P.op('pool', lambda e: e.iota(out=ti[:], pattern=[[1, 128]], base=1, channel_multiplier=0), reads=['ti32'], writes=['ti32'])
        P.op('dve', lambda e: e.tensor_copy(out=rowv[0][:], in_=ti[:]), reads=['ti32'], writes=['rowv0'])
        P.op('pool', lambda e: e.iota(out=ti[:], pattern=[[-1, 128]], base=128, channel_multiplier=0), reads=['ti32'], writes=['ti32'])
        P.op('dve', lambda e: e.tensor_copy(out=rowv[1][:], in_=ti[:]), reads=['ti32'], writes=['rowv1'])
        P.op('pool', lambda e: e.iota(out=ti[:, 0:1], pattern=[[1, 1]], base=127, channel_multiplier=-1), reads=['ti32'], writes=['ti32'])
        P.op('dve', lambda e: e.tensor_copy(out=colv[:, 0:1], in_=ti[:, 0:1]), reads=['ti32'], writes=['colv'])
        P.op('pool', lambda e: e.iota(out=ti[:, 0:1], pattern=[[1, 1]], base=0, channel_multiplier=1), reads=['ti32'], writes=['ti32'])
        P.op('dve', lambda e: e.tensor_copy(out=colv[:, 1:2], in_=ti[:, 0:1]), reads=['ti32'], writes=['colv'])
        bcast_load(C, lg[:, 0, :], d['ret_decay_f'][l, :], 'lg')
        bcast_load(C, lg[:, 1, :], d['ret_decay_b'][l, :], 'lg')
        P.op('act', lambda e: e.activation(out=lg[:], in_=lg[:], func=AF.Exp), reads=['lg'], writes=['lg'])
        P.op('dve', lambda e: e.tensor_scalar(out=lg[:], in0=lg[:], scalar1=-1.0, scalar2=None, op0=ALU.mult), reads=['lg'], writes=['lg'])
        P.op('act', lambda e: e.activation(out=cd[:], in_=lg[:], func=AF.Exp, scale=128.0), reads=['lg'], writes=['cd'])
        for dr in range(2):
            P.op('dve', lambda e: e.tensor_scalar(out=kd[:, dr, :], in0=lg[:, dr, :], scalar1=colv[:, dr:dr + 1], scalar2=None, op0=ALU.mult),
                 reads=['lg', 'colv'], writes=['kd'])
        P.op('act', lambda e: e.activation(out=kd[:], in_=kd[:], func=AF.Exp), reads=['kd'], writes=['kd'])

        NS = 2
        B = []
        for sl in range(NS):
            b = Ctx()
            b.KT = P.sb(st, "rKT%d" % sl, [128, ROWS], BF16)
            b.QT = P.sb(st, "rQT%d" % sl, [128, ROWS], BF16)
            b.Qd = P.sb(st, "rQd%d" % sl, [128, NTG, 128], BF16)
            b.Kt = P.sb(st, "rKt%d" % sl, [128, NTG, 128], BF16)
            b.Kd = P.sb(st, "rKd%d" % sl, [128, NTG, 128], BF16)
            b.V = P.sb(st, "rV%d" % sl, [128, NTG, 128], BF16)
            b.G = P.sb(st, "rG%d" % sl, [128, NTG, 128], BF16)
            b.yacc = P.sb(st, "ryacc%d" % sl, [128, NTG, 128], F32)
            b.DT = P.sb(st, "rDT%d" % sl, [128, 128], F32)
            b.qd = P.sb(st, "rqd%d" % sl, [128, 128], F32)
            b.S32 = P.sb(st, "rS32_%d" % sl, [128, 128], F32)
            b.S16 = P.sb(st, "rS16_%d" % sl, [128, 128], BF16)
            b.sT = [P.sb(st, "rsT%d_%d" % (sl, i), [128, 128], BF16) for i in range(2)]
            b.st6 = [P.sb(st, "rst6_%d_%d" % (sl, i), [128, 6], F32) for i in range(2)]
            b.mv = [P.sb(st, "rmv%d_%d" % (sl, i), [128, 2], F32) for i in range(2)]
            b.on = [P.sb(st, "ron%d_%d" % (sl, i), [128, 128], F32) for i in range(2)]
            b.yb = [P.sb(st, "ryb%d_%d" % (sl, i), [128, 128], BF16) for i in range(2)]
            b.yT = [P.sb(st, "ryT%d_%d" % (sl, i), [128, 128], BF16) for i in range(2)]
            b.sl = sl
            b.ps_s, b.ps_o, b.ps_u = C.pb[3 * sl], C.pb[3 * sl + 1], C.pb[3 * sl + 2]
            b.k = lambda n, b=b: "r%s%d" % (n, b.sl)
            B.append(b)
        row_chunks = [(0, 1024), (1024, 2048), (2048, 3072), (3072, 4096), (4096, ROWS)]
        hl = list(heads)
        for hp in range(0, len(hl), NS):
            grp = hl[hp:hp + NS]
            for b, h in zip(B, grp):
                b.h = h
                ko, vo, qo = KO['kr'] + h * 128, KO['vr'] + h * 128, QO['qr'] + h * 128
                for (r0, r1) in row_chunks:
                    P.dma('sp', b.KT[:, r0:r1], d['kv'][r0:r1, ko:ko + 128], reads=[('kv', t) for t in range(NTG)], writes=[b.k('KT')], transpose=True)
                    P.dma('sp', b.QT[:, r0:r1], d['qkv'][r0:r1, qo:qo + 128], reads=[('qkv', t) for t in range(NTG)], writes=[b.k('QT')], transpose=True)
                P.dma('sp', b.Kt[:], d['kv'][:, ko:ko + 128].rearrange("(t p) c -> p t c", p=128), reads=[('kv', t) for t in range(NTG)], writes=[b.k('Kt')])
                P.dma('sp', b.V[:], d['kv'][:, vo:vo + 128].rearrange("(t p) c -> p t c", p=128), reads=[('kv', t) for t in range(NTG)], writes=[b.k('V')])
            for dr in range(2):
                order = list(range(NTG)) if dr == 0 else [1, 0] + list(range(NTG - 1, 1, -1))
                for b in B[:len(grp)]:
                    h = b.h
                    go = (QO['gf'] if dr == 0 else QO['gb']) + h * 128
                    P.dma('sp', b.G[:], d['qkv'][:, go:go + 128].rearrange("(t p) c -> p t c", p=128), reads=[('qkv', t) for t in range(NTG)], writes=[b.k('G')])
                    P.op('act', lambda e: e.activation(out=b.DT[:], in_=pos[dr][:], func=AF.Exp, scale=lg[:, dr, h:h + 1]),
                         reads=['posd%d' % dr, 'lg'], writes=[b.k('DT')])
                    P.op('dve', lambda e: e.tensor_tensor(out=b.DT[:], in0=b.DT[:], in1=msk[dr][:], op=ALU.mult), reads=[b.k('DT'), 'mskd%d' % dr], writes=[b.k('DT')])
                    P.op('act', lambda e: e.activation(out=b.qd[:], in_=rowv[dr][:], func=AF.Exp, scale=lg[:, dr, h:h + 1]),
                         reads=['rowv%d' % dr, 'lg'], writes=[b.k('qd')])
                    P.op('dve', lambda e: e.tensor_tensor(out=b.Qd[:], in0=b.QT[:].rearrange("p (t c) -> p t c", c=128),
                                                          in1=b.qd[:].unsqueeze(1).to_broadcast([128, NTG, 128]), op=ALU.mult),
                         reads=[b.k('QT'), b.k('qd')], writes=[b.k('Qd')])
                    P.op('pool', lambda e: e.tensor_scalar(out=b.Kd[:], in0=b.Kt[:], scalar1=kd[:, dr, h:h + 1], scalar2=None, op0=ALU.mult),
                         reads=[b.k('Kt'), 'kd'], writes=[b.k('Kd')])
                    P.op('pool', lambda e: e.memset(b.S32[:], 0.0), writes=[b.k('S32')])
                    P.op('pool', lambda e: e.memset(b.S16[:], 0.0), writes=[b.k('S16')])
                for n, c in enumerate(order):
                    need_out = (c >= 2) or upd_ctx
                    cs = slice(c * 128, (c + 1) * 128)
                    for b in B[:len(grp)]:
                        h = b.h
                        if need_out:
                            i2 = n % 2
                            P.op('pe', lambda e: e.matmul(b.ps_s[:, 0:128], lhsT=b.KT[:, cs], rhs=b.QT[:, cs], start=True, stop=True),
                                 reads=[b.k('KT'), b.k('QT')], writes=[b.k('ps_s')])
                            P.op('dve', lambda e: e.tensor_tensor(out=b.sT[i2][:], in0=b.ps_s[:, 0:128], in1=b.DT[:], op=ALU.mult),
                                 reads=[b.k('ps_s'), b.k('DT')], writes=[b.k('sT%d' % i2)])
                            P.op('pe', lambda e: e.matmul(b.ps_o[:, 0:128], lhsT=b.sT[i2][:], rhs=b.V[:, c, :], start=True, stop=False),
                                 reads=[b.k('sT%d' % i2), b.k('V')], writes=[b.k('ps_o')])
                            P.op('pe', lambda e: e.matmul(b.ps_o[:, 0:128], lhsT=b.Qd[:, c, :], rhs=b.S16[:], start=False, stop=True),
                                 reads=[b.k('Qd'), b.k('S16')], writes=[b.k('ps_o')])
                            P.op('dve', lambda e: e.bn_stats(out=b.st6[i2][:], in_=b.ps_o[:, 0:128]), reads=[b.k('ps_o')], writes=[b.k('st6%d' % i2)])
                            P.op('dve', lambda e: e.bn_aggr(out=b.mv[i2][:], in_=b.st6[i2][:]), reads=[b.k('st6%d' % i2)], writes=[b.k('mv%d' % i2)])
                            P.op('dve', lambda e: e.tensor_scalar(out=b.mv[i2][:, 1:2], in0=b.mv[i2][:, 1:2], scalar1=EPS, scalar2=None, op0=ALU.add),
                                 reads=[b.k('mv%d' % i2)], writes=[b.k('mv%d' % i2)])
                            P.op('pool', lambda e: e.tensor_tensor(out=b.mv[i2][:, 1:2], in0=b.mv[i2][:, 1:2], in1=C.neghalf[:, 0:1], op=ALU.pow),
                                 reads=[b.k('mv%d' % i2), 'neghalf'], writes=[b.k('mv%d' % i2)])
                            P.op('dve', lambda e: e.tensor_scalar(out=b.on[i2][:], in0=b.ps_o[:, 0:128], scalar1=b.mv[i2][:, 0:1], scalar2=b.mv[i2][:, 1:2],
                                                                  op0=ALU.subtract, op1=ALU.mult),
                                 reads=[b.k('ps_o'), b.k('mv%d' % i2)], writes=[b.k('on%d' % i2)])
                            if dr == 0:
                                P.op('pool', lambda e: e.tensor_tensor(out=b.yacc[:, c, :], in0=b.on[i2][:], in1=b.G[:, c, :], op=ALU.mult),
                                     reads=[b.k('on%d' % i2), b.k('G')], writes=[b.k('yacc')])
                            else:
                                P.op('pool', lambda e: e.tensor_tensor(out=b.on[i2][:], in0=b.on[i2][:], in1=b.G[:, c, :], op=ALU.mult),
                                     reads=[b.k('on%d' % i2), b.k('G')], writes=[b.k('on%d' % i2)])
                                P.op('pool', lambda e: e.tensor_tensor(out=b.yb[i2][:], in0=b.on[i2][:], in1=b.yacc[:, c, :], op=ALU.add),
                                     reads=[b.k('on%d' % i2), b.k('yacc')], writes=[b.k('yb%d' % i2)])
                                bi = rot(C, 'pt', 2)
                                P.op('pe', lambda e: e.transpose(out=C.pt[bi][:, 0:128], in_=b.yb[i2][:], identity=C.ident[:]),
                                     reads=[b.k('yb%d' % i2), 'ident'], writes=['pt%d' % bi])
                                P.op('act', lambda e: e.activation(out=b.yT[i2][:], in_=C.pt[bi][:, 0:128], func=AF.Copy),
                                     reads=['pt%d' % bi], writes=[b.k('yT%d' % i2)])
                                P.dma('sp', d['catT'][(20 + h) * 128:(21 + h) * 128, cs], b.yT[i2][:], reads=[b.k('yT%d' % i2)], writes=[('catT', c)])
                        if n < len(order) - 1:
                            P.op('pe', lambda e: e.matmul(b.ps_u[:, 0:128], lhsT=b.Kd[:, c, :], rhs=b.V[:, c, :], start=True, stop=True),
                                 reads=[b.k('Kd'), b.k('V')], writes=[b.k('ps_u')])
                            P.op('dve', lambda e: e.scalar_tensor_tensor(out=b.S32[:], in0=b.S32[:], scalar=cd[:, dr, h:h + 1], in1=b.ps_u[:, 0:128],
                                                                         op0=ALU.mult, op1=ALU.add),
                                 reads=[b.k('S32'), b.k('ps_u'), 'cd'], writes=[b.k('S32')])
                            P.op('act', lambda e: e.activation(out=b.S16[:], in_=b.S32[:], func=AF.Copy), reads=[b.k('S32')], writes=[b.k('S16')])
        P.barrier()


def stage_attn(C, l, tiles, upd_ctx, heads=range(20)):
    P, d = C.P, C.d
    g0 = C.g0
    with ExitStack() as st:
        KT = [P.sb(st, "aKT%d" % i, [128, ROWS], BF16) for i in range(2)]
        KT2 = [P.sb(st, "aKT2_%d" % i, [128, ROWS], BF16) for i in range(2)]
        V = [P.sb(st, "aV%d" % i, [128, NTG, 128], BF16) for i in range(2)]
        QT = [P.sb(st, "aQT%d" % i, [128, TT], BF16) for i in range(2)]
        QT2 = [P.sb(st, "aQT2_%d" % i, [128, TT], BF16) for i in range(2)]
        PT = [P.sb(st, "aPT%d" % i, [128, 512], BF16) for i in range(3)]
        rec = [P.sb(st, "arec%d" % i, [128, 512], F32) for i in range(2)]
        row_chunks = [(0, 1024), (1024, 2048), (2048, 3072), (3072, 4096), (4096, ROWS)]
        allkv = [('kv', t) for t in range(NTG)]
        blocks = []
        lat = [t for t in tiles if t >= 2]
        if upd_ctx and any(t < 2 for t in tiles):
            blocks.append((0, 256, [0, 1]))
        for i in range(0, len(lat), 4):
            tt = lat[i:i + 4]
            blocks.append(((tt[0] - g0) * 128, len(tt) * 128, list(range(NTG))))
        ng = tiles[-1] + 1 - g0
        for hi in heads:
            if hi < 8:
                ko, vo, qo, scale, mla = KO['km'] + hi * 256, KO['vm'] + hi * 128, QO['qm'] + hi * 256, 192.0 ** -0.5, True
            else:
                hq = hi - 8
                ko, vo, qo, scale, mla = KO['kg'] + (hq // 3) * 128, KO['vg'] + (hq // 3) * 128, QO['qg'] + hq * 128, 128.0 ** -0.5, False
            si = rot(C, 'aset', 2)
            kT, kT2, v_, qT, qT2 = KT[si], KT2[si], V[si], QT[si], QT2[si]
            kk = lambda n: "a%s%d" % (n, si)
            for (r0, r1) in row_chunks:
                P.dma('sp', kT[:, r0:r1], d['kv'][r0:r1, ko:ko + 128], reads=allkv, writes=[kk('KT')], transpose=True)
                if mla:
                    P.dma('sp', kT2[:, r0:r1], d['kv'][r0:r1, ko + 128:ko + 256], reads=allkv, writes=[kk('KT2')], transpose=True)
            P.dma('sp', v_[:], d['kv'][:, vo:vo + 128].rearrange("(t p) c -> p t c", p=128), reads=allkv, writes=[kk('V')])
            qr0, qr1 = g0 * 128, (g0 + ng) * 128
            P.dma('sp', qT[:, 0:ng * 128], d['qkv'][qr0:qr1, qo:qo + 128], reads=[('qkv', t) for t in range(NTG)], writes=[kk('QT')], transpose=True)
            if mla:
                P.dma('sp', qT2[:, 0:ng * 128], d['qkv'][qr0:qr1, qo + 128:qo + 256], reads=[('qkv', t) for t in range(NTG)], writes=[kk('QT2')], transpose=True)
            for (q0, nq, kts) in blocks:
                oi = rot(C, 'aO', 2)
                psO, psD = C.pb[2 + oi], C.pb[4 + oi]
                kO, kD = 'pb%d' % (2 + oi), 'pb%d' % (4 + oi)

                def emitS(i):
                    kt = kts[i]
                    ps = C.pb[i % 2]
                    P.op('pe', lambda e: e.matmul(ps[:, 0:nq], lhsT=kT[:, kt * 128:(kt + 1) * 128], rhs=qT[:, q0:q0 + nq], start=True, stop=not mla),
                         reads=[kk('KT'), kk('QT')], writes=['pb%d' % (i % 2)])
                    if mla:
                        P.op('pe', lambda e: e.matmul(ps[:, 0:nq], lhsT=kT2[:, kt * 128:(kt + 1) * 128], rhs=qT2[:, q0:q0 + nq], start=False, stop=True),
                             reads=[kk('KT2'), kk('QT2')], writes=['pb%d' % (i % 2)])
                emitS(0)
                nk = len(kts)
                for i, kt in enumerate(kts):
                    if i + 1 < nk:
                        emitS(i + 1)
                    pi = rot(C, 'aPT', 3)
                    P.op('act', lambda e: e.activation(out=PT[pi][:, 0:nq], in_=C.pb[i % 2][:, 0:nq], func=AF.Exp, scale=scale),
                         reads=['pb%d' % (i % 2)], writes=['aPT%d' % pi])
                    P.op('pe', lambda e: e.matmul(psO[:, 0:nq], lhsT=v_[:, kt, :], rhs=PT[pi][:, 0:nq], start=(i == 0), stop=(i == nk - 1)),
                         reads=[kk('V'), 'aPT%d' % pi], writes=[kO])
                    P.op('pe', lambda e: e.matmul(psD[:, 0:nq], lhsT=C.ones[:], rhs=PT[pi][:, 0:nq], start=(i == 0), stop=(i == nk - 1)),
                         reads=['ones', 'aPT%d' % pi], writes=[kD])
                ri = rot(C, 'arec', 2)
                P.op('dve', lambda e: e.reciprocal(out=rec[ri][:, 0:nq], in_=psD[:, 0:nq]), reads=[kD], writes=['arec%d' % ri])
                P.op('dve', lambda e: e.tensor_tensor(out=C.big3[:, hi, q0:q0 + nq], in0=psO[:, 0:nq], in1=rec[ri][:, 0:nq], op=ALU.mult),
                     reads=[kO, 'arec%d' % ri], writes=[('cat', hi)])
        P.barrier()


def stage_wout(C, l, tiles, xsrc):
    P, d = C.P, C.d
    g0 = C.g0
    with ExitStack() as st:
        C.wbuf = [P.sb(st, "wbuf%d" % i, [128, 32, 512], BF16) for i in range(2)]
        GA = [P.sb(st, "GA%d" % r, [128, D], F32) for r in range(2)]
        xt = [P.sb(st, "xo%d" % i, [128, 512], F32) for i in range(3)]
        tmp = [P.sb(st, "xtmp%d" % i, [128, 512], F32) for i in range(3)]
        rows_needed = [0, 1] if any(t < 2 for t in tiles) else [0]
        for r in rows_needed:
            mod_bcast(C, GA[r][:], l, r, 2 * D, D, 'GA%d' % r)
        t0, t1 = tiles[0], tiles[-1] + 1
        P.dma('sp', C.big3[:, 20:32, (t0 - g0) * 128:(t1 - g0) * 128],
              d['catT'][20 * 128:32 * 128, t0 * 128:t1 * 128].rearrange("(k p) t -> p k t", p=128),
              reads=[('catT', c) for c in range(NTG)], writes=[('cat', 20)])

        def evac(ci):
            def f(t, ps, pskey):
                r = 1 if t < 2 else 0
                rows, cols = slice(t * 128, (t + 1) * 128), slice(ci * 512, (ci + 1) * 512)
                xi = rot(C, 'xo', 3)
                P.dma('sp', xt[xi][:], xsrc[rows, cols], reads=[('x', t)], writes=['xo%d' % xi])
                P.op('dve', lambda e: e.tensor_tensor(out=tmp[xi][:], in0=ps[:], in1=GA[r][:, cols], op=ALU.mult),
                     reads=[pskey, 'GA%d' % r], writes=['xtmp%d' % xi])
                P.op('pool', lambda e: e.tensor_tensor(out=tmp[xi][:], in0=tmp[xi][:], in1=xt[xi][:], op=ALU.add),
                     reads=['xtmp%d' % xi, 'xo%d' % xi], writes=['xtmp%d' % xi])
                P.dma('sp', d['x1'][rows, cols], tmp[xi][:], reads=['xtmp%d' % xi], writes=[('x1', t)])
            return f
        items = [dict(W=d['w_out'][l], n0=ci * 512, nw=512, KC=32,
                      lhsT=lambda kc, t: C.big3[:, kc, (t - g0) * 128:(t - g0 + 1) * 128],
                      tiles=list(tiles), rkeys=lambda t: [('cat', h) for h in range(21)], evac=evac(ci)) for ci in range(8)]
        run_items(C, items)
        P.barrier()


def alloc_routing(C, st):
    P = C.P
    R = Ctx()
    R.posm = P.sb(st, "Rposm", [128, NTG, NE], F32)
    R.i1 = P.sb(st, "Ri1", [128, NTG], I32)
    R.i2 = P.sb(st, "Ri2", [128, NTG], I32)
    R.w1 = P.sb(st, "Rw1", [128, NTG], F32)
    R.w2 = P.sb(st, "Rw2", [128, NTG], F32)
    R.st_scores = ExitStack()
    R.scores = P.sb(R.st_scores, "Rscores", [128, NTG, NE], F32)
    P.op('pool', lambda e: e.memset(R.scores[:], 0.0), writes=['scores'])
    return R


def stage_route(C, l, tiles, R):
    P, d = C.P, C.d
    lts = list(tiles)
    scores, posm = R.scores, R.posm
    with ExitStack() as st:
        wgt = P.sb(st, "Rwgt", [128, NTG, NE], F32)
        oh1 = P.sb(st, "Roh1", [128, NTG, NE], F32)
        oh2 = P.sb(st, "Roh2", [128, NTG, NE], F32)
        sel = P.sb(st, "rsel", [128, NTG, NE], F32)
        tA = P.sb(st, "rtA", [128, NTG, NE], F32)
        tB = P.sb(st, "rtB", [128, NTG, NE], F32)
        mask = P.sb(st, "rmask", [128, NTG, NE], F32)
        maskb = P.sb(st, "rmaskb", [128, NTG, NE], BF16)
        gs = P.sb(st, "rgs", [128, NTG * 4], F32)
        g1 = P.sb(st, "rg1", [128, NTG * 4], F32)
        mx = P.sb(st, "rmx", [128, NTG], F32)
        brb = P.sb(st, "brb", [128, NE], F32)
        tri = P.sb(st, "tri", [128, 128], BF16)
        trif = P.sb(st, "trif", [128, 128], F32)
        offs = P.sb(st, "roffs", [128, NTG, NE], F32)
        ie32 = P.sb(st, "ie32", [128, NE], I32)
        ief = P.sb(st, "ief", [128, NE], F32)
        bcast_load(C, brb[:], d['b_router'], 'brb')
        P.op('pool', lambda e: e.memset(trif[:], 1.0), writes=['trif'])
        P.op('pool', lambda e: e.affine_select(out=trif[:], in_=trif[:], pattern=[[1, 128]], compare_op=ALU.is_gt, fill=0.0, base=0, channel_multiplier=-1),
             reads=['trif'], writes=['trif'])
        P.op('dve', lambda e: e.tensor_copy(out=tri[:], in_=trif[:]), reads=['trif'], writes=['tri'])
        P.op('pool', lambda e: e.iota(out=ie32[:], pattern=[[CAP, NE]], base=0, channel_multiplier=0), writes=['ie32'])
        P.op('dve', lambda e: e.tensor_copy(out=ief[:], in_=ie32[:]), reads=['ie32'], writes=['ief'])

        def dv(fn, reads, writes):
            P.op('dve', fn, reads=reads, writes=writes)
        dv(lambda e: e.tensor_tensor(out=sel[:], in0=scores[:], in1=brb[:].unsqueeze(1).to_broadcast([128, NTG, NE]), op=ALU.add), ['scores', 'brb'], ['rsel'])
        s4 = sel[:].rearrange("p t (g x) -> p (t g) x", x=4)
        pairs = [(0, 1), (0, 2), (0, 3), (1, 2), (1, 3), (2, 3)]
        for pi, (a_, bb) in enumerate(pairs):
            tgt = gs if pi == 0 else g1
            dv(lambda e: e.tensor_tensor(out=tgt[:], in0=s4[:, :, a_], in1=s4[:, :, bb], op=ALU.add), ['rsel'], ['rgs' if pi == 0 else 'rg1'])
            if pi > 0:
                dv(lambda e: e.tensor_tensor(out=gs[:], in0=gs[:], in1=g1[:], op=ALU.max), ['rgs', 'rg1'], ['rgs'])
        gs3 = gs[:].rearrange("p (t g) -> p t g", g=4)
        dv(lambda e: e.tensor_reduce(out=mx[:], in_=gs3, axis=AX.X, op=ALU.max), ['rgs'], ['rmx'])
        g13 = g1[:].rearrange("p (t g) -> p t g", g=4)
        dv(lambda e: e.tensor_tensor(out=g13, in0=gs3, in1=mx[:].unsqueeze(2).to_broadcast([128, NTG, 4]), op=ALU.is_equal), ['rgs', 'rmx'], ['rg1'])
        dv(lambda e: e.tensor_scalar(out=g1[:], in0=g1[:], scalar1=1e4, scalar2=-1e4, op0=ALU.mult, op1=ALU.add), ['rg1'], ['rg1'])
        tA4 = tA[:].rearrange("p t (g x) -> p (t g) x", x=4)
        dv(lambda e: e.tensor_tensor(out=tA4, in0=s4, in1=g1[:].unsqueeze(2).to_broadcast([128, NTG * 4, 4]), op=ALU.add), ['rsel', 'rg1'], ['rtA'])
        dv(lambda e: e.tensor_reduce(out=mx[:], in_=tA[:], axis=AX.X, op=ALU.max), ['rtA'], ['rmx'])
        dv(lambda e: e.tensor_tensor(out=oh1[:], in0=tA[:], in1=mx[:].unsqueeze(2).to_broadcast([128, NTG, NE]), op=ALU.is_equal), ['rtA', 'rmx'], ['roh1'])
        dv(lambda e: e.scalar_tensor_tensor(out=tB[:], in0=oh1[:], scalar=-1e4, in1=tA[:], op0=ALU.mult, op1=ALU.add), ['roh1', 'rtA'], ['rtB'])
        dv(lambda e: e.tensor_reduce(out=mx[:], in_=tB[:], axis=AX.X, op=ALU.max), ['rtB'], ['rmx'])
        dv(lambda e: e.tensor_tensor(out=oh2[:], in0=tB[:], in1=mx[:].unsqueeze(2).to_broadcast([128, NTG, NE]), op=ALU.is_equal), ['rtB', 'rmx'], ['roh2'])
        for lt_ in range(NTG):
            if lt_ not in lts:
                P.op('pool', lambda e: e.memset(oh1[:, lt_, :], 0.0), reads=['roh1'], writes=['roh1'])
                P.op('pool', lambda e: e.memset(oh2[:, lt_, :], 0.0), reads=['roh2'], writes=['roh2'])
        dv(lambda e: e.tensor_tensor(out=mask[:], in0=oh1[:], in1=oh2[:], op=ALU.add), ['roh1', 'roh2'], ['rmask'])
        dv(lambda e: e.tensor_tensor(out=wgt[:], in0=scores[:], in1=mask[:], op=ALU.mult), ['scores', 'rmask'], ['rwgt'])
        dv(lambda e: e.tensor_reduce(out=mx[:], in_=wgt[:], axis=AX.X, op=ALU.add), ['rwgt'], ['rmx'])
        dv(lambda e: e.tensor_scalar(out=mx[:], in0=mx[:], scalar1=1e-30, scalar2=None, op0=ALU.max), ['rmx'], ['rmx'])
        dv(lambda e: e.reciprocal(out=mx[:], in_=mx[:]), ['rmx'], ['rmx'])
        dv(lambda e: e.tensor_tensor(out=wgt[:], in0=wgt[:], in1=mx[:].unsqueeze(2).to_broadcast([128, NTG, NE]), op=ALU.mult), ['rwgt', 'rmx'], ['rwgt'])
        dv(lambda e: e.tensor_copy(out=maskb[:], in_=mask[:]), ['rmask'], ['rmaskb'])
        mb2 = maskb[:].rearrange("p t e -> p (t e)")
        HW = NTG * NE // 2
        posf = posm[:].rearrange("p t e -> p (t e)")
        tBf = tB[:].rearrange("p t e -> p (t e)")
        for hh in range(2):
            psP, psC = C.pb[2 + 2 * hh], C.pb[3 + 2 * hh]
            cs_ = slice(hh * HW, (hh + 1) * HW)
            P.op('pe', lambda e: e.matmul(psP[:, 0:HW], lhsT=tri[:], rhs=mb2[:, cs_], start=True, stop=True), reads=['tri', 'rmaskb'], writes=['pb%d' % (2 + 2 * hh)])
            P.op('pe', lambda e: e.matmul(psC[:, 0:HW], lhsT=C.ones[:], rhs=mb2[:, cs_], start=True, stop=True), reads=['ones', 'rmaskb'], writes=['pb%d' % (3 + 2 * hh)])
            dv(lambda e: e.tensor_copy(out=tBf[:, cs_], in_=psC[:, 0:HW]), ['pb%d' % (3 + 2 * hh)], ['rtB'])
            dv(lambda e: e.tensor_copy(out=posf[:, cs_], in_=psP[:, 0:HW]), ['pb%d' % (2 + 2 * hh)], ['rposm'])
        P.op('pool', lambda e: e.memset(offs[:], 0.0), writes=['roffs'])
        for k in range(1, NTG):
            dv(lambda e: e.tensor_tensor(out=offs[:, k, :], in0=offs[:, k - 1, :], in1=tB[:, k - 1, :], op=ALU.add), ['roffs', 'rtB'], ['roffs'])
        dv(lambda e: e.tensor_tensor(out=posm[:], in0=posm[:], in1=offs[:], op=ALU.add), ['rposm', 'roffs'], ['rposm'])
        dv(lambda e: e.tensor_tensor(out=posm[:], in0=posm[:], in1=mask[:], op=ALU.mult), ['rposm', 'rmask'], ['rposm'])
        dv(lambda e: e.tensor_tensor(out=posm[:], in0=posm[:], in1=mask[:], op=ALU.add), ['rposm', 'rmask'], ['rposm'])
        dv(lambda e: e.tensor_scalar(out=posm[:], in0=posm[:], scalar1=-1.0, scalar2=None, op0=ALU.add), ['rposm'], ['rposm'])
        dv(lambda e: e.tensor_tensor(out=tA[:], in0=posm[:], in1=ief[:].unsqueeze(1).to_broadcast([128, NTG, NE]), op=ALU.add), ['rposm', 'ief'], ['rtA'])
        for (oh, ohk, it_, itk, w_, wk) in ((oh1, 'roh1', R.i1, 'Ri1', R.w1, 'Rw1'), (oh2, 'roh2', R.i2, 'Ri2', R.w2, 'Rw2')):
            dv(lambda e: e.tensor_tensor(out=tB[:], in0=tA[:], in1=oh[:], op=ALU.mult), ['rtA', ohk], ['rtB'])
            dv(lambda e: e.tensor_reduce(out=mx[:], in_=tB[:], axis=AX.X, op=ALU.add), ['rtB'], ['rmx'])
            dv(lambda e: e.tensor_copy(out=it_[:], in_=mx[:]), ['rmx'], [itk])
            dv(lambda e: e.tensor_tensor(out=tB[:], in0=wgt[:], in1=oh[:], op=ALU.mult), ['rwgt', ohk], ['rtB'])
            dv(lambda e: e.tensor_reduce(out=w_[:], in_=tB[:], axis=AX.X, op=ALU.add), ['rtB'], [wk])
        P.barrier()


def stage_ffn_a(C, l, tiles, R):
    P, d = C.P, C.d
    g0 = C.g0
    stage_norm(C, l, d['x1'], 'g_ffn', 3, 4, tiles, tok_dst=d['h2tok'], router=dict(scores=R.scores))
    with ExitStack() as st:
        C.wbuf = [P.sb(st, "wbuf%d" % i, [128, 32, 512], BF16) for i in range(2)]
        GFc = [[P.sb(st, "GF%d_%d" % (r, i), [128, 512], F32) for i in range(2)] for r in range(2)]
        rows_needed = [0, 1] if any(t < 2 for t in tiles) else [0]
        atok = [P.sb(st, "atok%d" % i, [128, 512], BF16) for i in range(2)]
        xt = [P.sb(st, "fx%d" % i, [128, 512], F32) for i in range(2)]
        tmp = [P.sb(st, "ftmp%d" % i, [128, 512], F32) for i in range(2)]
        sg = P.sb(st, "sgs", [128, NT, 512], BF16)
        aT = P.sb(st, "aTs", [128, 8, TT], BF16)
        C.atok = atok

        def mkx(ci):
            state = {}

            def f(t, ps, pskey):
                if 'gi' not in state:
                    state['gi'] = rot(C, 'GFc', 2)
                    for r_ in rows_needed:
                        mod_bcast(C, GFc[r_][state['gi']][:], l, r_, 5 * D + ci * 512, 512, 'GF%d_%d' % (r_, state['gi']))
                gi = state['gi']
                r = 1 if t < 2 else 0
                rows, cols = slice(t * 128, (t + 1) * 128), slice(ci * 512, (ci + 1) * 512)
                xi = rot(C, 'fx', 2)
                P.dma('sp', xt[xi][:], d['x1'][rows, cols], reads=[('x1', t)], writes=['fx%d' % xi])
                P.op('dve', lambda e: e.tensor_tensor(out=tmp[xi][:], in0=ps[:], in1=GFc[r][gi][:], op=ALU.mult),
                     reads=[pskey, 'GF%d_%d' % (r, gi)], writes=['ftmp%d' % xi])
                P.op('pool', lambda e: e.tensor_tensor(out=tmp[xi][:], in0=tmp[xi][:], in1=xt[xi][:], op=ALU.add),
                     reads=['ftmp%d' % xi, 'fx%d' % xi], writes=['ftmp%d' % xi])
                P.dma('sp', d['x1'][rows, cols], tmp[xi][:], reads=['ftmp%d' % xi], writes=[('x1', t)])
            return f
        big_l = lambda kc, t: C.big3[:, kc, (t - g0) * 128:(t - g0 + 1) * 128]
        idx_t = lambda t: t - g0
        items = []
        for ci in range(2):
            items.append(dict(W=d['s_gate'][l], n0=ci * 512, nw=512, KC=32, lhsT=big_l, tiles=list(tiles), rkeys=lambda t: [('big', t)],
                              evac=evac_gate(C, sg, idx_t)))
            items.append(dict(W=d['s_up'][l], n0=ci * 512, nw=512, KC=32, lhsT=big_l, tiles=list(tiles), rkeys=lambda t: [('big', t)],
                              evac=evac_up(C, sg, aT, ci, idx_t, 'aTs')))
        for ci in range(8):
            items.append(dict(W=d['s_down'][l], n0=ci * 512, nw=512, KC=8, lhsT=lambda kc, t: aT[:, kc, (t - g0) * 128:(t - g0 + 1) * 128],
                              tiles=list(tiles), rkeys=lambda t: ['aTs'], evac=mkx(ci)))
        run_items(C, items)
        P.barrier()


def evac_gate(C, sgbuf, idx):
    def f(t, ps, pskey):
        C.P.op('act', lambda e: e.activation(out=sgbuf[:, idx(t), 0:512], in_=ps[:], func=AF.Silu), reads=[pskey], writes=['sg'])
    return f


def evac_up(C, sgbuf, aTbuf, ci, idx, akey):
    def f(t, ps, pskey):
        ai = rot(C, 'atok', 2)
        atok = C.atok
        C.P.op('dve', lambda e: e.tensor_tensor(out=atok[ai][:], in0=ps[:], in1=sgbuf[:, idx(t), 0:512], op=ALU.mult),
               reads=[pskey, 'sg'], writes=['atok%d' % ai])

        def dst_(i, g, src, bkey):
            copy_alt(C, aTbuf[:, ci * 4 + i:ci * 4 + i + g, idx(t) * 128:(idx(t) + 1) * 128], src.rearrange("p (a b) -> p a b", b=128), [bkey], [akey])
        transpose_to(C, lambda i: atok[ai][:, i * 128:(i + 1) * 128], 4, dst_, ['atok%d' % ai], None)
    return f


def pool_gather(C, out, src, idx_ap, reads, writes):
    P = C.P
    k = P.di % NDMA
    P.di += 1
    toks = P._deps(reads, writes)
    if P.dcnt[k] > 0:
        toks.append((P.dsem[k], 16 * P.dcnt[k]))
    P._emit_waits('pool', toks)
    ins = C.nc.gpsimd.indirect_dma_start(out=out, out_offset=None, in_=src, in_offset=bass.IndirectOffsetOnAxis(ap=idx_ap, axis=0))
    P.dcnt[k] += 1
    ins.then_inc(P.dsem[k], 16)
    tok = (P.dsem[k], 16 * P.dcnt[k])
    P._commit(tok, reads, writes)
    P.n_ops += 1


def stage_ffn_b(C, l, tiles, R, dst, dst_row0):
    P, d = C.P, C.d
    lts = list(tiles)
    NS = CAP // 128
    posm = R.posm
    with ExitStack() as st:
        C.wbuf = [P.sb(st, "wbuf%d" % i, [128, 32, 512], BF16) for i in range(2)]
        atok = [P.sb(st, "atok%d" % i, [128, 512], BF16) for i in range(2)]
        C.atok = atok
        ystg = [P.sb(st, "ystg%d" % i, [128, 512], BF16) for i in range(1)]
        iof = P.sb(st, "iof", [128, CAP], F32)
        tvi = P.sb(st, "tvi", [128, NTG, 2], I32)
        tv = P.sb(st, "tv", [128, NTG, 2], BF16)
        selb = P.sb(st, "selb", [128, CAP], BF16)
        idT = P.sb(st, "idT", [2, CAP], BF16)
        idf = [P.sb(st, "idf%d" % i, [128, 2], F32) for i in range(2)]
        idi = [P.sb(st, "idi%d" % i, [128, 1], I32) for i in range(NS)]
        xtok = [P.sb(st, "xtok%d" % i, [128, D], BF16) for i in range(2)]
        xsT = P.sb(st, "xsT", [128, 32, CAP], BF16)
        sgE = P.sb(st, "sgE", [128, NS, 512], BF16)
        aTE = P.sb(st, "aTE", [128, 8, CAP], BF16)
        P.op('pool', lambda e: e.iota(out=iof[:], pattern=[[1, CAP]], base=0, channel_multiplier=0, allow_small_or_imprecise_dtypes=True), writes=['iof'])
        P.op('pool', lambda e: e.iota(out=tvi[:].rearrange("p t c -> p (t c)"), pattern=[[1, NTG], [0, 2]], base=0, channel_multiplier=0), writes=['tvi'])
        P.op('dve', lambda e: e.tensor_copy(out=tv[:, :, 0:1], in_=tvi[:, :, 0:1]), reads=['tvi'], writes=['tv'])
        P.op('pool', lambda e: e.iota(out=tvi[:].rearrange("p t c -> p (t c)"), pattern=[[0, NTG * 2]], base=0, channel_multiplier=1), reads=['tvi', 'tv'], writes=['tvi'])
        P.op('dve', lambda e: e.tensor_copy(out=tv[:, :, 1:2], in_=tvi[:, :, 1:2]), reads=['tvi'], writes=['tv'])
        colch = [(c0, min(512, CAP - c0)) for c0 in range(0, CAP, 512)]

        def disp_idx(ex):
            for n_, lt in enumerate(lts):
                P.op('dve', lambda e: e.tensor_scalar(out=selb[:], in0=iof[:], scalar1=posm[:, lt, ex:ex + 1], scalar2=None, op0=ALU.is_equal),
                     reads=['iof', 'rposm'], writes=['selb'])
                for ci_, (c0, cw) in enumerate(colch):
                    P.op('pe', lambda e: e.matmul(C.pb[2 + ci_][0:2, 0:cw], lhsT=tv[:, lt, :], rhs=selb[:, c0:c0 + cw], start=(n_ == 0), stop=(n_ == len(lts) - 1)),
                         reads=['selb', 'tv'], writes=['pb%d' % (2 + ci_)])
            for ci_, (c0, cw) in enumerate(colch):
                P.op('act', lambda e: e.activation(out=idT[:, c0:c0 + cw], in_=C.pb[2 + ci_][0:2, 0:cw], func=AF.Copy), reads=['pb%d' % (2 + ci_)], writes=['idT'])
            for st_ in range(NS):
                bi = rot(C, 'pt', 2)
                P.op('pe', lambda e: e.transpose(out=C.pt[bi][:, 0:2], in_=idT[0:2, st_ * 128:(st_ + 1) * 128], identity=C.ident[0:2, 0:2]),
                     reads=['idT', 'ident'], writes=['pt%d' % bi])
                fi = rot(C, 'idf', 2)
                P.op('dve', lambda e: e.tensor_copy(out=idf[fi][:], in_=C.pt[bi][:, 0:2]), reads=['pt%d' % bi], writes=['idf%d' % fi])
                P.op('dve', lambda e: e.scalar_tensor_tensor(out=idf[fi][:, 0:1], in0=idf[fi][:, 0:1], scalar=128.0, in1=idf[fi][:, 1:2], op0=ALU.mult, op1=ALU.add),
                     reads=['idf%d' % fi], writes=['idf%d' % fi])
                P.op('dve', lambda e: e.tensor_copy(out=idi[st_][:], in_=idf[fi][:, 0:1]), reads=['idf%d' % fi], writes=['idi%d' % st_])

        def disp_gather(st_):
            xi = st_ % 2
            pool_gather(C, xtok[xi][:, :], d['h2tok'][:, :], idi[st_][:, :], ['idi%d' % st_] + [('h2tok', lt) for lt in range(NTG)], ['xtok%d' % xi])

        def disp_tr(st_):
            xi = st_ % 2

            def dst_(i, g, src, bkey):
                copy_alt(C, xsT[:, i:i + g, st_ * 128:(st_ + 1) * 128], src.rearrange("p (a b) -> p a b", b=128), [bkey], ['xsT'])
            transpose_to(C, lambda i: xtok[xi][:, i * 128:(i + 1) * 128], 32, dst_, ['xtok%d' % xi], None)

        def chain(*fns):
            def f():
                for fn in fns:
                    fn()
            return f
        items = []
        for ex in range(NE):
            xl = lambda kc, t: xsT[:, kc, t * 128:(t + 1) * 128]
            ident_t = lambda t: t
            nxt = ex + 1
            for ci in range(2):
                it = dict(W=d['e_gate'][l, ex], n0=ci * 512, nw=512, KC=32, lhsT=xl, tiles=list(range(NS)), rkeys=lambda t: ['xsT'],
                          evac=evac_gate(C, sgE, ident_t))
                if ci == 0:
                    def pre(ex=ex):
                        disp_idx(ex)
                        disp_gather(0)
                        for s_ in range(NS):
                            if s_ + 1 < NS:
                                disp_gather(s_ + 1)
                            disp_tr(s_)
                    it['pre'] = pre
                items.append(it)
                it2 = dict(W=d['e_up'][l, ex], n0=ci * 512, nw=512, KC=32, lhsT=xl, tiles=list(range(NS)), rkeys=lambda t: ['xsT'],
                           evac=evac_up(C, sgE, aTE, ci, ident_t, 'aTE'))
                if ci == 1 and nxt < NE and PIPE_DISPATCH:
                    it2['post'] = (lambda nxt=nxt: disp_idx(nxt))
                items.append(it2)

            def mky(ci, ex=ex):
                def f(t, ps, pskey):
                    yi = 0
                    copy_alt(C, ystg[yi][:], ps[:], [pskey], ['ystg%d' % yi])
                    P.dma('sp', d['yall'][ex * CAP + t * 128:ex * CAP + (t + 1) * 128, ci * 512:(ci + 1) * 512], ystg[yi][:], reads=['ystg%d' % yi], writes=['yall'])
                return f
            for ci in range(8):
                it3 = dict(W=d['e_down'][l, ex], n0=ci * 512, nw=512, KC=8, lhsT=lambda kc, t: aTE[:, kc, t * 128:(t + 1) * 128],
                           tiles=list(range(NS)), rkeys=lambda t: ['aTE'], evac=mky(ci))
                if nxt < NE and PIPE_DISPATCH:
                    if ci == 0:
                        it3['pre'] = (lambda: disp_gather(0))
                    else:
                        it3['pre'] = chain(lambda ci=ci: disp_gather(ci), lambda ci=ci: disp_tr(ci - 1))
                items.append(it3)
        run_items(C, items)
        P.barrier()
    with ExitStack() as st:
        GF = [P.sb(st, "GFf%d" % r, [128, D], F32) for r in range(2)]
        y1 = [P.sb(st, "y1_%d" % i, [128, D], BF16) for i in range(2)]
        y2 = [P.sb(st, "y2_%d" % i, [128, D], BF16) for i in range(2)]
        xa = [P.sb(st, "xa%d" % i, [128, D], F32) for i in range(2)]
        ta = [P.sb(st, "ta%d" % i, [128, D], F32) for i in range(2)]
        rows_needed = [0, 1] if any(t < 2 for t in tiles) else [0]
        for r in rows_needed:
            mod_bcast(C, GF[r][:], l, r, 5 * D, D, 'GFf%d' % r)
        for t in tiles:
            lt = t
            r = 1 if t < 2 else 0
            bi = rot(C, 'cmb', 2)
            pool_gather(C, y1[bi][:, :], d['yall'][:, :], R.i1[:, lt:lt + 1], ['Ri1', 'yall'], ['y1_%d' % bi])
            pool_gather(C, y2[bi][:, :], d['yall'][:, :], R.i2[:, lt:lt + 1], ['Ri2', 'yall'], ['y2_%d' % bi])
            P.dma('sp', xa[bi][:], d['x1'][t * 128:(t + 1) * 128, :], reads=[('x1', t)], writes=['xa%d' % bi])
            P.op('dve', lambda e: e.tensor_scalar(out=ta[bi][:], in0=y1[bi][:], scalar1=R.w1[:, lt:lt + 1], scalar2=None, op0=ALU.mult),
                 reads=['y1_%d' % bi, 'Rw1'], writes=['ta%d' % bi])
            P.op('dve', lambda e: e.scalar_tensor_tensor(out=ta[bi][:], in0=y2[bi][:], scalar=R.w2[:, lt:lt + 1], in1=ta[bi][:], op0=ALU.mult, op1=ALU.add),
                 reads=['y2_%d' % bi, 'Rw2', 'ta%d' % bi], writes=['ta%d' % bi])
            P.op('pool', lambda e: e.tensor_tensor(out=ta[bi][:], in0=ta[bi][:], in1=GF[r][:], op=ALU.mult), reads=['ta%d' % bi, 'GFf%d' % r], writes=['ta%d' % bi])
            P.op('pool', lambda e: e.tensor_tensor(out=ta[bi][:], in0=ta[bi][:], in1=xa[bi][:], op=ALU.add), reads=['ta%d' % bi, 'xa%d' % bi], writes=['ta%d' % bi])
            P.dma('sp', dst[(t - dst_row0) * 128:(t - dst_row0 + 1) * 128, :], ta[bi][:], reads=['ta%d' % bi], writes=[('xout', t)])
        P.barrier()


WEIGHT_SPECS = {
    'g_mix': ([2, D], F32), 'g_ffn': ([2, D], F32), 'w_mod': ([2, D, 6 * D], F32), 'b_mod': ([2, 6 * D], F32),
    'w_in': ([2, D, DIN], F32),
    'mla_q_lora_g': ([2, 1024], F32), 'mla_kv_lora_g': ([2, 512], F32),
    'w_uq': ([2, 1024, 1536], F32), 'w_ukv': ([2, 512, 2048], F32),
    'mla_q_g': ([2, 192], F32), 'mla_k_g': ([2, 192], F32), 'gqa_q_g': ([2, 128], F32), 'gqa_k_g': ([2, 128], F32),
    'ret_decay_f': ([2, 12], F32), 'ret_decay_b': ([2, 12], F32), 'w_out': ([2, D, D], F32),
    'w_router': ([D, NE], F32), 'b_router': ([NE], F32),
    'e_gate': ([2, NE, D, 1024], F32), 'e_up': ([2, NE, D, 1024], F32), 'e_down': ([2, NE, 1024, D], F32),
    's_gate': ([2, D, 1024], F32), 's_up': ([2, D, 1024], F32), 's_down': ([2, 1024, D], F32),
}
OTHER_SPECS = {
    'xin': ([ROWS, D], F32), 'cT': ([128, 32, 2], F32),
    'rope_mla': ([ROWS, 64], F32), 'rope_gqa': ([ROWS, 128], F32), 'rope_ret': ([ROWS, 128], F32),
    'modv': ([2, 2, 6 * D], F32),
    'lr': ([ROWS, 1600], F32), 'qkv': ([ROWS, QW], BF16), 'kv': ([ROWS, KVW], BF16),
    'catT': ([D, ROWS], BF16),
    'x1': ([ROWS, D], F32), 'xres': ([ROWS, D], F32), 'h2tok': ([ROWS, D], BF16), 'yall': ([NE * CAP, D], BF16),
    'y': ([4096, D], F32), 'dbgr': ([128, 3, NT * NE], F32),
}


class DramSet(dict):
    def __init__(self, nc, ext_in, ext_out):
        super().__init__()
        self.nc, self.ext_in, self.ext_out = nc, set(ext_in), set(ext_out)

    def __missing__(self, name):
        spec = WEIGHT_SPECS.get(name) or OTHER_SPECS[name]
        if name in self.ext_in:
            kind = "ExternalInput"
        elif name in self.ext_out:
            kind = "ExternalOutput"
        else:
            kind = "Internal"
        ap = self.nc.dram_tensor(name, list(spec[0]), spec[1], kind=kind).ap()
        self[name] = ap
        return ap


def new_ctx(ext_in, ext_out):
    nc = bass.Bass("TRN2", target_bir_lowering=False)
    C = Ctx()
    C.nc = nc
    C.gst = ExitStack()
    C.P = Prog(nc, C.gst)
    C.d = DramSet(nc, ext_in, ext_out)
    C.rotc = {}
    C.g0 = 0
    emit_consts(C)
    return C


def alloc_big(C, st):
    C.big = C.P.sb(st, "big", [128, 32 * TT], BF16)
    C.big3 = C.big[:].rearrange("p (k t) -> p k t", t=TT)


def phase_A(C, l, groups=GROUPS):
    xsrc = C.d['xin'] if l == 0 else C.d['xres']
    with ExitStack() as st:
        alloc_big(C, st)
        for (g0, g1) in groups:
            C.g0 = g0
            tiles = list(range(g0, g1))
            stage_norm(C, l, xsrc, 'g_mix', 0, 1, tiles)
            stage_win(C, l, tiles)
            stage_mla(C, l, tiles)
    C.g0 = 0


def rope_tables():
    f32 = np.float32
    s = np.arange(4096).astype(f32)
    row, col = np.floor(s / 64).astype(f32), np.mod(s, 64).astype(f32)

    def freqs(dim):
        return (f32(10000.0) ** (-(np.arange(0, dim, 2, dtype=f32)) / f32(dim))).astype(f32)

    def tab(ang_lat, half, ctx_ang=None):
        t = np.zeros((ROWS, 2 * half), f32)
        t[:256, :half] = 1.0
        if ctx_ang is not None:
            t[:256, :half] = np.cos(ctx_ang)
            t[:256, half:] = np.sin(ctx_ang)
        t[256:, :half] = np.cos(ang_lat)
        t[256:, half:] = np.sin(ang_lat)
        return t
    fm, fg, fr = freqs(32), freqs(64), freqs(128)
    am = np.concatenate([row[:, None] * fm, col[:, None] * fm], axis=1).astype(f32)
    ag = np.concatenate([row[:, None] * fg, col[:, None] * fg], axis=1).astype(f32)
    ar = ((f32(256) + s)[:, None] * fr).astype(f32)
    arc = (np.arange(256, dtype=f32)[:, None] * fr).astype(f32)
    return tab(am, 32), tab(ag, 64), tab(ar, 64, arc)


def core_inputs(inputs, b, names):
    m = {}
    for k in names:
        if k in WEIGHT_SPECS:
            m[k] = np.ascontiguousarray(inputs[k])
    if 'xin' in names:
        m['xin'] = np.ascontiguousarray(np.concatenate([inputs['ctx'][b], inputs['x'][b]], axis=0))
    if 'cT' in names:
        cc = np.stack([inputs['c'][b], inputs['c_ctx']], axis=1)
        m['cT'] = np.ascontiguousarray(cc.reshape(32, 128, 2).transpose(1, 0, 2))
    if 'rope_mla' in names:
        m['rope_mla'], m['rope_gqa'], m['rope_ret'] = rope_tables()
    return m


def phase_mix(C, l, upd_ctx, groups=GROUPS, ffn=True):
    xsrc = C.d['xin'] if l == 0 else C.d['xres']
    with ExitStack() as stl:
        R = alloc_routing(C, stl)
        all_tiles = []
        for (g0, g1) in groups:
            C.g0 = g0
            tiles = [t for t in range(g0, g1) if (t >= 2 or upd_ctx)]
            all_tiles += tiles
            with ExitStack() as st:
                alloc_big(C, st)
                stage_attn(C, l, tiles, upd_ctx)
                stage_wout(C, l, tiles, xsrc)
                if ffn:
                    stage_ffn_a(C, l, tiles, R)
        C.g0 = 0
        if not ffn:
            R.st_scores.close()
        if ffn:
            stage_route(C, l, all_tiles, R)
            R.st_scores.close()
            if l == 0:
                stage_ffn_b(C, l, all_tiles, R, C.d['xres'], 0)
            else:
                stage_ffn_b(C, l, all_tiles, R, C.d['y'], 2)


LAYER_WEIGHTS = ['g_mix', 'g_ffn', 'w_in', 'mla_q_lora_g', 'mla_kv_lora_g', 'w_uq', 'w_ukv', 'mla_q_g', 'mla_k_g', 'gqa_q_g', 'gqa_k_g',
                 'ret_decay_f', 'ret_decay_b', 'w_out', 'w_router', 'b_router', 'e_gate', 'e_up', 'e_down', 's_gate', 's_up', 's_down']
FULL_INPUTS = ['xin', 'cT', 'rope_mla', 'rope_gqa', 'rope_ret', 'w_mod', 'b_mod'] + LAYER_WEIGHTS


def build_full():
    C = new_ctx(FULL_INPUTS, ['y'])
    stage_mod(C)
    for l in range(2):
        upd = (l == 0)
        phase_A(C, l)
        stage_ret(C, l, upd)
        phase_mix(C, l, upd)
    C.P.finish()
    C.gst.close()
    return C.nc


def kernel(**inputs):
    inputs = {k: np.asarray(v) for k, v in inputs.items()}
    nc = build_full()
    in_maps = [core_inputs(inputs, b, FULL_INPUTS) for b in range(2)]
    res = run_bass_kernel_spmd(nc, in_maps, core_ids=[0, 1])
    return np.stack([np.asarray(res.results[b]['y'], dtype=np.float32) for b in range(2)], axis=0)
```

```python
from contextlib import ExitStack
import numpy as np
import ml_dtypes
import concourse.bass as bass
import concourse.mybir as mybir
from concourse.bass_utils import run_bass_kernel_spmd

F32 = mybir.dt.float32
BF16 = mybir.dt.bfloat16
I32 = mybir.dt.int32
AF = mybir.ActivationFunctionType
ALU = mybir.AluOpType
AX = mybir.AxisListType

NDMA = 24
SAME_ENGINE_SYNC = True
PIPE_DISPATCH = False
WSTEP = 8
D = 4096
NT = 10
TT = 1280
NTG = 34
ROWS = 4352
GROUPS = [(0, 10), (10, 18), (18, 26), (26, 34)]
DIN = 11840
EPS = 1e-6
QW = 8192
KVW = 7168
QO = dict(qm=0, qg=2048, qr=3584, gf=5120, gb=6656)
KO = dict(km=0, vm=2048, kg=3072, vg=3584, kr=4096, vr=5632)
CAP = 1280
NE = 16

W_CHUNKS = [(0, 512, 'cq', 0), (512, 512, 'cq', 1), (1024, 512, 'ckv', 0), (1536, 64, 'kpe', 0)]
_off = 1600
for _k, _n in [('gq', 3), ('gk', 1), ('gv', 1), ('rq', 3), ('rk', 3), ('rv', 3), ('gf', 3), ('gb', 3)]:
    for _i in range(_n):
        W_CHUNKS.append((_off, 512, _k, _i))
        _off += 512


class Prog:
    def __init__(self, nc, stack):
        self.nc = nc
        self.eng = {'pe': nc.tensor, 'act': nc.scalar, 'dve': nc.vector,
                    'pool': nc.gpsimd, 'sp': nc.sync}
        self.sem = {e: stack.enter_context(nc.semaphore("s_" + e)) for e in self.eng}
        self.cnt = {e: 0 for e in self.eng}
        self.seen = {e: {} for e in self.eng}
        self.dsem = [stack.enter_context(nc.semaphore("d%d" % i)) for i in range(NDMA)]
        self.dcnt = [0] * NDMA
        self.di = 0
        self.last_w = {}
        self.readers = {}
        self.n_ops = 0
        self.uid = 0

    def sb(self, st, name, shape, dt):
        self.uid += 1
        return st.enter_context(self.nc.sbuf_tensor("%s_%d" % (name, self.uid), list(shape), dt))

    def ps(self, st, name, shape, dt):
        return st.enter_context(self.nc.psum_tensor(name, list(shape), dt))

    def _deps(self, reads, writes):
        toks = []
        for k in reads:
            t = self.last_w.get(k)
            if t is not None:
                toks.append(t)
        for k in writes:
            t = self.last_w.get(k)
            if t is not None:
                toks.append(t)
            toks.extend(self.readers.get(k, ()))
        return toks

    def _emit_waits(self, e, toks):
        best = {}
        for (s, v) in toks:
            if v > best.get(id(s), (None, 0))[1]:
                best[id(s)] = (s, v)
        seen = self.seen[e]
        for sid, (s, v) in best.items():
            if s is self.sem[e] and (e == 'pe' or not SAME_ENGINE_SYNC):
                continue
            if seen.get(sid, 0) >= v:
                continue
            self.eng[e].wait_ge(s, v)
            seen[sid] = v

    def _commit(self, tok, reads, writes):
        for k in writes:
            self.last_w[k] = tok
            self.readers[k] = []
        for k in reads:
            self.readers.setdefault(k, []).append(tok)

    def op(self, e, fn, reads=(), writes=()):
        self._emit_waits(e, self._deps(reads, writes))
        ins = fn(self.eng[e])
        self.cnt[e] += 1
        ins.then_inc(self.sem[e], 1)
        tok = (self.sem[e], self.cnt[e])
        self._commit(tok, reads, writes)
        self.n_ops += 1
        return tok

    def dma(self, e, out, in_, reads=(), writes=(), **kw):
        k = self.di % NDMA
        self.di += 1
        toks = self._deps(reads, writes)
        if self.dcnt[k] > 0:
            toks.append((self.dsem[k], 16 * self.dcnt[k]))
        self._emit_waits(e, toks)
        ins = self.eng[e].dma_start(out=out, in_=in_, **kw)
        self.dcnt[k] += 1
        ins.then_inc(self.dsem[k], 16)
        tok = (self.dsem[k], 16 * self.dcnt[k])
        self._commit(tok, reads, writes)
        self.n_ops += 1
        return tok

    def barrier(self):
        toks = [(self.sem[e], self.cnt[e]) for e in self.eng if self.cnt[e] > 0]
        toks += [(self.dsem[k], 16 * self.dcnt[k]) for k in range(NDMA) if self.dcnt[k] > 0]
        for e in self.eng:
            self._emit_waits(e, toks)

    def finish(self):
        toks = [(self.sem[e], self.cnt[e]) for e in self.eng if self.cnt[e] > 0]
        toks += [(self.dsem[k], 16 * self.dcnt[k]) for k in range(NDMA) if self.dcnt[k] > 0]
        self._emit_waits('sp', toks)
        self.eng['sp'].nop()


class Ctx:
    pass


def rot(C, name, n):
    i = C.rotc.get(name, 0)
    C.rotc[name] = i + 1
    return i % n


def emit_consts(C):
    P, st = C.P, C.gst
    C.identf = P.sb(st, "identf", [128, 128], F32)
    C.ident = P.sb(st, "ident", [128, 128], BF16)
    C.ones = P.sb(st, "ones", [128, 128], BF16)
    C.neghalf = P.sb(st, "neghalf", [128, 16], F32)
    P.op('pool', lambda e: e.memset(C.identf[:], 1.0), writes=['identf'])
    P.op('pool', lambda e: e.affine_select(out=C.identf[:], in_=C.identf[:], pattern=[[-1, 128]],
                                           compare_op=ALU.is_equal, fill=0.0, base=0, channel_multiplier=1),
         reads=['identf'], writes=['identf'])
    P.op('dve', lambda e: e.tensor_copy(out=C.ident[:], in_=C.identf[:]), reads=['identf'], writes=['ident'])
    P.op('pool', lambda e: e.memset(C.ones[:], 1.0), writes=['ones'])
    P.op('pool', lambda e: e.memset(C.neghalf[:], -0.5), writes=['neghalf'])
    C.pb = [P.ps(st, "pb%d" % i, [128, 512], F32) for i in range(6)]
    C.pt = [P.ps(st, "pt%d" % i, [128, 1024], BF16) for i in range(2)]


def rstd_from_ss(C, ss_ap, n, dim, key, outkey):
    P = C.P
    P.op('dve', lambda e: e.tensor_scalar(out=ss_ap, in0=ss_ap, scalar1=1.0 / dim, scalar2=EPS, op0=ALU.mult, op1=ALU.add),
         reads=[key], writes=[key])
    P.op('pool', lambda e: e.tensor_tensor(out=ss_ap, in0=ss_ap, in1=C.neghalf[:, 0:n], op=ALU.pow),
         reads=[key, 'neghalf'], writes=[key])


def transpose_to(C, src_fn, nblk, dst_fn, rkeys, wkeys, dt=BF16):
    P = C.P
    grp = 8 if dt == BF16 else 4
    i = 0
    while i < nblk:
        g = min(grp, nblk - i)
        if dt == BF16:
            bi = rot(C, 'pt', 2)
            bank, bkey = C.pt[bi], 'pt%d' % bi
            ident = C.ident
        else:
            bi = 4 + rot(C, 'ptf', 2)
            bank, bkey = C.pb[bi], 'pb%d' % bi
            ident = C.identf
        for q in range(g):
            P.op('pe', lambda e: e.transpose(out=bank[:, q * 128:(q + 1) * 128], in_=src_fn(i + q), identity=ident[:]),
                 reads=rkeys + ['ident', 'identf'], writes=[bkey])
        dst_fn(i, g, bank[:, 0:g * 128], bkey)
        i += g


def copy_alt(C, out, in_, reads, writes):
    P = C.P
    if rot(C, 'cpalt', 2) == 0:
        P.op('act', lambda e: e.activation(out=out, in_=in_, func=AF.Copy), reads=reads, writes=writes)
    else:
        P.op('dve', lambda e: e.tensor_copy(out=out, in_=in_), reads=reads, writes=writes)


def bcast_load(C, dst, src_row, key, eng='sp'):
    C.P.dma(eng, dst, src_row.partition_broadcast(128), writes=[key])


def mod_bcast(C, dst, l, r, n0, n, key):
    C.P.dma('sp', dst[:, 0:n], C.d['modv'][l, r, n0:n0 + n].partition_broadcast(128), reads=['modv'], writes=[key])


def run_items(C, items):
    P = C.P

    def load(it):
        s = rot(C, 'wslot', 2)
        it['slot'] = s
        KC, nw, n0, W = it['KC'], it['nw'], it['n0'], it['W']
        step = WSTEP
        for k0 in range(0, KC, step):
            k1 = min(KC, k0 + step)
            P.dma('pool', C.wbuf[s][:, k0:k1, 0:nw],
                  W[k0 * 128:k1 * 128, n0:n0 + nw].rearrange("(kc p) n -> p kc n", p=128),
                  writes=['wb%d_%d' % (s, k0 // step)])

    n = len(items)
    for i in range(min(2, n)):
        load(items[i])
    for i, it in enumerate(items):
        if it.get('pre'):
            it['pre']()
        s = it['slot']
        for t in it['tiles']:
            bi = rot(C, 'psA', 2)
            ps, pskey = C.pb[bi], 'pb%d' % bi
            KC = it['KC']
            for kc in range(KC):
                P.op('pe', lambda e: e.matmul(ps[:, 0:it['nw']], lhsT=it['lhsT'](kc, t), rhs=C.wbuf[s][:, kc, 0:it['nw']],
                                              start=(kc == 0), stop=(kc == KC - 1)),
                     reads=it['rkeys'](t) + ['wb%d_%d' % (s, kc // WSTEP)], writes=[pskey])
            it['evac'](t, ps, pskey)
        if i + 2 < n:
            load(items[i + 2])
        if it.get('post'):
            it['post']()


def stage_mod(C):
    P, d = C.P, C.d
    with ExitStack() as st:
        cT = P.sb(st, "cT", [128, 32, 2], F32)
        bsb = [P.sb(st, "bsb%d" % i, [2, 512], F32) for i in range(2)]
        wf = [P.sb(st, "wf%d" % i, [128, 8, 512], F32) for i in range(4)]
        mo = [P.sb(st, "mo%d" % i, [2, 512], F32) for i in range(2)]
        P.dma('sp', cT[:], d['cT'][:, :, :], writes=['cT'])
        P.op('act', lambda e: e.activation(out=cT[:], in_=cT[:], func=AF.Silu), reads=['cT'], writes=['cT'])
        for l in range(2):
            for ci in range(48):
                n0 = ci * 512
                ps, pskey = C.pb[ci % 2], 'pb%d' % (ci % 2)
                for k0 in range(4):
                    wi = rot(C, 'wf', 4)
                    P.dma('sp', wf[wi][:], d['w_mod'][l, k0 * 1024:(k0 + 1) * 1024, n0:n0 + 512].rearrange("(kc p) n -> p kc n", p=128),
                          writes=['wf%d' % wi])
                    for kk in range(8):
                        kc = k0 * 8 + kk
                        P.op('pe', lambda e: e.matmul(ps[0:2, :], lhsT=cT[:, kc, :], rhs=wf[wi][:, kk, :], start=(kc == 0), stop=(kc == 31)),
                             reads=['cT', 'wf%d' % wi], writes=[pskey])
                mi = rot(C, 'mo', 2)
                P.dma('sp', bsb[mi][:], d['b_mod'][l, n0:n0 + 512].partition_broadcast(2), writes=['bsb%d' % mi])
                P.op('dve', lambda e: e.tensor_tensor(out=mo[mi][:], in0=ps[0:2, :], in1=bsb[mi][:], op=ALU.add),
                     reads=[pskey, 'bsb%d' % mi], writes=['mo%d' % mi])
                P.dma('sp', d['modv'][l, :, n0:n0 + 512], mo[mi][:], reads=['mo%d' % mi], writes=['modv'])
        P.barrier()


def stage_norm(C, l, xsrc, gname, cs_sh, cs_sc, tiles, tok_dst=None, router=None):
    P, d = C.P, C.d
    with ExitStack() as st:
        A = [P.sb(st, "A%d" % r, [128, D], F32) for r in range(2)]
        Bv = [P.sb(st, "B%d" % r, [128, D], F32) for r in range(2)]
        xt = [P.sb(st, "xt%d" % i, [128, D], F32) for i in range(2 if router is None else 1)]
        hb = [P.sb(st, "hb%d" % i, [128, D], BF16) for i in range(2 if router is None else 1)]
        ss = [P.sb(st, "ss%d" % i, [128, 1], F32) for i in range(2)]
        gm = xt[-1]
        gmk = 'xt%d' % (len(xt) - 1)
        bcast_load(C, gm[:], d[gname][l, :], gmk)
        rows = [0, 1] if any(t < 2 for t in tiles) else [0]
        for r in rows:
            mod_bcast(C, A[r][:], l, r, cs_sc * D, D, 'A%d' % r)
            mod_bcast(C, Bv[r][:], l, r, cs_sh * D, D, 'B%d' % r)
            P.op('dve', lambda e: e.scalar_tensor_tensor(out=A[r][:], in0=A[r][:], scalar=1.0, in1=gm[:], op0=ALU.add, op1=ALU.mult),
                 reads=['A%d' % r, gmk], writes=['A%d' % r])
        if router is not None:
            hl = [P.sb(st, "hl%d" % i, [128, D], BF16) for i in range(1)]
            hlT = [P.sb(st, "hlT%d" % i, [128, 32, 128], BF16) for i in range(1)]
            wrh = P.sb(st, "wrh", [128, 32, NE], BF16)
            wrl = P.sb(st, "wrl", [128, 32, NE], BF16)
            wr = xt[0][:, 0:32 * NE].rearrange("p (k n) -> p k n", n=NE)
            P.dma('sp', wr, d['w_router'].rearrange("(kc p) n -> p kc n", p=128), writes=['xt0'])
            P.op('dve', lambda e: e.tensor_copy(out=wrh[:], in_=wr), reads=['xt0'], writes=['wrh'])
            P.op('dve', lambda e: e.tensor_tensor(out=wrl[:], in0=wr, in1=wrh[:], op=ALU.subtract), reads=['xt0', 'wrh'], writes=['wrl'])
        for t in tiles:
            r = 1 if t < 2 else 0
            lt = t - C.g0
            xi = rot(C, 'xt', 2)
            x_, xk = xt[xi % len(xt)], 'xt%d' % (xi % len(xt))
            h_, hk = hb[xi % len(hb)], 'hb%d' % (xi % len(hb))
            s_, sk = ss[xi], 'ssn%d' % xi
            P.dma('sp', x_[:], xsrc[t * 128:(t + 1) * 128, :], reads=[('x', t)], writes=[xk])
            P.op('act', lambda e: e.activation(out=h_[:], in_=x_[:], func=AF.Square, accum_out=s_[:]), reads=[xk], writes=[hk, sk])
            rstd_from_ss(C, s_[:], 1, D, sk, sk)
            P.op('dve', lambda e: e.scalar_tensor_tensor(out=x_[:], in0=x_[:], scalar=s_[:, 0:1], in1=A[r][:], op0=ALU.mult, op1=ALU.mult),
                 reads=[xk, sk, 'A%d' % r], writes=[xk])
            if router is None:
                P.op('pool', lambda e: e.tensor_tensor(out=h_[:], in0=x_[:], in1=Bv[r][:], op=ALU.add),
                     reads=[xk, 'B%d' % r], writes=[hk])
            else:
                P.op('pool', lambda e: e.tensor_tensor(out=x_[:], in0=x_[:], in1=Bv[r][:], op=ALU.add),
                     reads=[xk, 'B%d' % r], writes=[xk])
                P.op('act', lambda e: e.activation(out=h_[:], in_=x_[:], func=AF.Copy), reads=[xk], writes=[hk])
            if tok_dst is not None:
                P.dma('sp', tok_dst[t * 128:(t + 1) * 128, :], h_[:], reads=[hk], writes=[('h2tok', t)])
            def dst(i, g, src, bkey):
                copy_alt(C, C.big3[:, i:i + g, lt * 128:(lt + 1) * 128], src.rearrange("p (a b) -> p a b", b=128),
                         [bkey], [('big', t)])
            transpose_to(C, lambda i: h_[:, i * 128:(i + 1) * 128], 32, dst, [hk], None)
            if router is not None:
                li = 0
                P.op('dve', lambda e: e.tensor_tensor(out=hl[li][:], in0=x_[:], in1=h_[:], op=ALU.subtract), reads=[xk, hk], writes=['hl%d' % li])

                def dstl(i, g, src, bkey):
                    copy_alt(C, hlT[li][:, i:i + g, :], src.rearrange("p (a b) -> p a b", b=128), [bkey], ['hlT%d' % li])
                transpose_to(C, lambda i: hl[li][:, i * 128:(i + 1) * 128], 32, dstl, ['hl%d' % li], None)
                psr, psrk = C.pb[3], 'pb3'
                n_mm = 0
                for kc in range(32):
                    for (lh, lkey, rw, rkey) in ((C.big3[:, kc, lt * 128:(lt + 1) * 128], ('big', t), wrh, 'wrh'),
                                                 (hlT[li][:, kc, :], 'hlT%d' % li, wrh, 'wrh'),
                                                 (C.big3[:, kc, lt * 128:(lt + 1) * 128], ('big', t), wrl, 'wrl')):
                        P.op('pe', lambda e: e.matmul(psr[:, 0:NE], lhsT=lh, rhs=rw[:, kc, :], start=(n_mm == 0), stop=(n_mm == 95)),
                             reads=[lkey, rkey], writes=[psrk])
                        n_mm += 1
                P.op('act', lambda e: e.activation(out=router['scores'][:, t, :], in_=psr[:, 0:NE], func=AF.Sigmoid),
                     reads=[psrk], writes=['scores'])
        P.barrier()


def stage_win(C, l, tiles):
    P, d = C.P, C.d
    with ExitStack() as st:
        C.wbuf = [P.sb(st, "wbuf%d" % i, [128, 32, 512], BF16) for i in range(2)]
        stgf = [P.sb(st, "stgf%d" % i, [128, 512], F32) for i in range(3)]
        stgb = [P.sb(st, "stgb%d" % i, [128, 512], BF16) for i in range(4)]
        xs = [P.sb(st, "xs%d" % i, [128, 512], F32) for i in range(3)]
        tm = [P.sb(st, "tm%d" % i, [128, 512], F32) for i in range(3)]
        ssq = [P.sb(st, "ssq%d" % i, [128, 4], F32) for i in range(3)]
        tr = [[P.sb(st, "tr%d_%d" % (i, q), [128, 256], F32) for q in range(4)] for i in range(2)]
        gq = P.sb(st, "gqg", [128, 128], F32)
        gk = P.sb(st, "gkg", [128, 128], F32)
        ropeg = P.sb(st, "ropeg", [128, NT, 128], F32)
        roper = P.sb(st, "roper", [128, NT, 128], F32)
        bcast_load(C, gq[:], d['gqa_q_g'][l, :], 'gqg')
        bcast_load(C, gk[:], d['gqa_k_g'][l, :], 'gkg')
        g0, ng = C.g0, len(tiles)
        P.dma('sp', ropeg[:, 0:ng, :], d['rope_gqa'][g0 * 128:(g0 + ng) * 128, :].rearrange("(t p) c -> p t c", p=128), writes=['ropeg'])
        P.dma('sp', roper[:, 0:ng, :], d['rope_ret'][g0 * 128:(g0 + ng) * 128, :].rearrange("(t p) c -> p t c", p=128), writes=['roper'])

        def evac(ci):
            n0, nw, kind, sub = W_CHUNKS[ci]

            def f(t, ps, pskey):
                rows = slice(t * 128, (t + 1) * 128)
                if kind in ('cq', 'ckv', 'kpe'):
                    si = rot(C, 'stgf', 3)
                    P.op('act', lambda e: e.activation(out=stgf[si][:, 0:nw], in_=ps[:, 0:nw], func=AF.Copy),
                         reads=[pskey], writes=['stgf%d' % si])
                    P.dma('sp', d['lr'][rows, n0:n0 + nw], stgf[si][:, 0:nw], reads=['stgf%d' % si], writes=[('lr', t)])
                    return
                si = rot(C, 'stgb', 4)
                sb_, sbk = stgb[si], 'stgb%d' % si
                if kind in ('gv', 'rv', 'gf', 'gb'):
                    fn = AF.Silu if kind in ('gf', 'gb') else AF.Copy
                    P.op('act', lambda e: e.activation(out=sb_[:], in_=ps[:], func=fn), reads=[pskey], writes=[sbk])
                    if kind == 'gv':
                        dst, dk = d['kv'][rows, KO['vg']:KO['vg'] + 512], ('kv', t)
                    elif kind == 'rv':
                        dst, dk = d['kv'][rows, KO['vr'] + sub * 512:KO['vr'] + (sub + 1) * 512], ('kv', t)
                    elif kind == 'gf':
                        dst, dk = d['qkv'][rows, QO['gf'] + sub * 512:QO['gf'] + (sub + 1) * 512], ('qkv', t)
                    else:
                        dst, dk = d['qkv'][rows, QO['gb'] + sub * 512:QO['gb'] + (sub + 1) * 512], ('qkv', t)
                    P.dma('sp', dst, sb_[:], reads=[sbk], writes=[dk])
                    return
                xi = rot(C, 'xs', 3)
                x_, xk = xs[xi], 'xs%d' % xi
                scale = (128.0 ** -0.5) if kind == 'rk' else 1.0
                P.op('act', lambda e: e.activation(out=x_[:], in_=ps[:], func=AF.Copy, scale=scale), reads=[pskey], writes=[xk])
                x3 = x_[:].rearrange("p (h c) -> p h c", c=128)
                if kind in ('gq', 'gk'):
                    t_, tk = tm[xi], 'tm%d' % xi
                    s_, sk = ssq[xi], 'ssq%d' % xi
                    P.op('dve', lambda e: e.tensor_tensor(out=t_[:], in0=x_[:], in1=x_[:], op=ALU.mult), reads=[xk], writes=[tk])
                    P.op('dve', lambda e: e.tensor_reduce(out=s_[:], in_=t_[:].rearrange("p (h c) -> p h c", c=128), axis=AX.X, op=ALU.add),
                         reads=[tk], writes=[sk])
                    rstd_from_ss(C, s_[:], 4, 128, sk, sk)
                    P.op('dve', lambda e: e.tensor_tensor(out=x3, in0=x3, in1=s_[:].unsqueeze(2).to_broadcast([128, 4, 128]), op=ALU.mult),
                         reads=[xk, sk], writes=[xk])
                    g_ = gq if kind == 'gq' else gk
                    P.op('dve', lambda e: e.tensor_tensor(out=x3, in0=x3, in1=g_[:].unsqueeze(1).to_broadcast([128, 4, 128]), op=ALU.mult),
                         reads=[xk, 'gqg', 'gkg'], writes=[xk])
                    rp = ropeg
                else:
                    rp = roper
                cosb = rp[:, t - C.g0, 0:64].unsqueeze(1).to_broadcast([128, 4, 64])
                sinb = rp[:, t - C.g0, 64:128].unsqueeze(1).to_broadcast([128, 4, 64])
                x1, x2 = x3[:, :, 0:64], x3[:, :, 64:128]
                o3 = sb_[:].rearrange("p (h c) -> p h c", c=128)
                ri = rot(C, 'tr', 2)
                tA, tB, tC, tD = [tr[ri][q][:].rearrange("p (h c) -> p h c", c=64) for q in range(4)]
                kA, kB, kC, kD = ['tr%d_%d' % (ri, q) for q in range(4)]
                P.op('dve', lambda e: e.tensor_tensor(out=tA, in0=x1, in1=cosb, op=ALU.mult), reads=[xk, 'ropeg', 'roper'], writes=[kA])
                P.op('pool', lambda e: e.tensor_tensor(out=tB, in0=x2, in1=sinb, op=ALU.mult), reads=[xk, 'ropeg', 'roper'], writes=[kB])
                P.op('pool', lambda e: e.tensor_tensor(out=tC, in0=x1, in1=sinb, op=ALU.mult), reads=[xk, 'ropeg', 'roper'], writes=[kC])
                P.op('dve', lambda e: e.tensor_tensor(out=tD, in0=x2, in1=cosb, op=ALU.mult), reads=[xk, 'ropeg', 'roper'], writes=[kD])
                P.op('dve', lambda e: e.tensor_tensor(out=o3[:, :, 0:64], in0=tA, in1=tB, op=ALU.subtract), reads=[kA, kB], writes=[sbk])
                P.op('pool', lambda e: e.tensor_tensor(out=o3[:, :, 64:128], in0=tC, in1=tD, op=ALU.add), reads=[kC, kD], writes=[sbk])
                if kind == 'gq':
                    dst, dk = d['qkv'][rows, QO['qg'] + sub * 512:QO['qg'] + (sub + 1) * 512], ('qkv', t)
                elif kind == 'gk':
                    dst, dk = d['kv'][rows, KO['kg']:KO['kg'] + 512], ('kv', t)
                elif kind == 'rq':
                    dst, dk = d['qkv'][rows, QO['qr'] + sub * 512:QO['qr'] + (sub + 1) * 512], ('qkv', t)
                else:
                    dst, dk = d['kv'][rows, KO['kr'] + sub * 512:KO['kr'] + (sub + 1) * 512], ('kv', t)
                P.dma('sp', dst, sb_[:], reads=[sbk], writes=[dk])
            return f

        items = []
        for ci, (n0, nw, kind, sub) in enumerate(W_CHUNKS):
            items.append(dict(W=d['w_in'][l], n0=n0, nw=nw, KC=32, lhsT=lambda kc, t: C.big3[:, kc, (t - C.g0) * 128:(t - C.g0 + 1) * 128],
                              tiles=list(tiles), rkeys=lambda t: [('big', t)], evac=evac(ci)))
        run_items(C, items)
        P.barrier()


def stage_mla(C, l, tiles):
    P, d = C.P, C.d
    with ExitStack() as st:
        wuq = P.sb(st, "wuq", [128, 8, 1536], BF16)
        wukv = P.sb(st, "wukv", [128, 4, 2048], BF16)
        gql = P.sb(st, "gql", [128, 1024], F32)
        gkvl = P.sb(st, "gkvl", [128, 512], F32)
        gmq = P.sb(st, "gmq", [128, 192], F32)
        gmk = P.sb(st, "gmk", [128, 192], F32)
        ropem = P.sb(st, "ropem", [128, NT, 64], F32)
        lrt = [P.sb(st, "lrt%d" % i, [128, 1600], F32) for i in range(2)]
        junk = P.sb(st, "junk", [128, 1024], F32)
        nb = [P.sb(st, "nb%d" % i, [128, 1024], BF16) for i in range(2)]
        cT = [P.sb(st, "cT%d" % i, [128, 8, 128], BF16) for i in range(2)]
        ss1 = [P.sb(st, "ss1_%d" % i, [128, 4], F32) for i in range(4)]
        spb = [P.sb(st, "spb%d" % i, [128, 1], F32) for i in range(2)]
        xs = [P.sb(st, "xsm%d" % i, [128, 512], F32) for i in range(3)]
        tm = [P.sb(st, "tmm%d" % i, [128, 512], F32) for i in range(3)]
        kpn = [P.sb(st, "kpn%d" % i, [128, 64], F32) for i in range(3)]
        tr = [[P.sb(st, "trm%d_%d" % (i, q), [128, 64], F32) for q in range(4)] for i in range(3)]
        og = [P.sb(st, "og%d" % i, [128, 512], BF16) for i in range(4)]
        ov = [P.sb(st, "ov%d" % i, [128, 256], BF16) for i in range(3)]
        P.dma('pool', wuq[:], d['w_uq'][l].rearrange("(kc p) n -> p kc n", p=128), writes=['wuq'])
        P.dma('pool', wukv[:], d['w_ukv'][l].rearrange("(kc p) n -> p kc n", p=128), writes=['wukv'])
        bcast_load(C, gql[:], d['mla_q_lora_g'][l, :], 'gql')
        bcast_load(C, gkvl[:], d['mla_kv_lora_g'][l, :], 'gkvl')
        bcast_load(C, gmq[:], d['mla_q_g'][l, :], 'gmq')
        bcast_load(C, gmk[:], d['mla_k_g'][l, :], 'gmk')
        g0, ng = C.g0, len(tiles)
        P.dma('sp', ropem[:, 0:ng, :], d['rope_mla'][g0 * 128:(g0 + ng) * 128, :].rearrange("(t p) c -> p t c", p=128), writes=['ropem'])
        for i in range(4):
            P.op('pool', lambda e: e.memset(og[i][:], 0.0), writes=['og%d' % i])

        def rope32(x1, x2, o1, o2, t, rkeys, wkey, nh):
            cosb = ropem[:, t - C.g0, 0:32].unsqueeze(1).to_broadcast([128, nh, 32])
            sinb = ropem[:, t - C.g0, 32:64].unsqueeze(1).to_broadcast([128, nh, 32])
            ri = rot(C, 'trm', 3)
            tA, tB, tC, tD = [tr[ri][q][:, 0:nh * 32].rearrange("p (h c) -> p h c", c=32) for q in range(4)]
            kA, kB, kC, kD = ['trm%d_%d' % (ri, q) for q in range(4)]
            P.op('dve', lambda e: e.tensor_tensor(out=tA, in0=x1, in1=cosb, op=ALU.mult), reads=rkeys + ['ropem'], writes=[kA])
            P.op('pool', lambda e: e.tensor_tensor(out=tB, in0=x2, in1=sinb, op=ALU.mult), reads=rkeys + ['ropem'], writes=[kB])
            P.op('pool', lambda e: e.tensor_tensor(out=tC, in0=x1, in1=sinb, op=ALU.mult), reads=rkeys + ['ropem'], writes=[kC])
            P.op('dve', lambda e: e.tensor_tensor(out=tD, in0=x2, in1=cosb, op=ALU.mult), reads=rkeys + ['ropem'], writes=[kD])
            P.op('dve', lambda e: e.tensor_tensor(out=o1, in0=tA, in1=tB, op=ALU.subtract), reads=[kA, kB], writes=[wkey])
            P.op('pool', lambda e: e.tensor_tensor(out=o2, in0=tC, in1=tD, op=ALU.add), reads=[kC, kD], writes=[wkey])

        for t in tiles:
            rows = slice(t * 128, (t + 1) * 128)
            li = rot(C, 'lrt', 2)
            lr_, lk = lrt[li], 'lrt%d' % li
            P.dma('sp', lr_[:], d['lr'][rows, :], reads=[('lr', t)], writes=[lk])
            si = rot(C, 'ss1', 4)
            s_, sk = ss1[si], 'ss1_%d' % si
            P.op('act', lambda e: e.activation(out=junk[:, 0:1024], in_=lr_[:, 0:1024], func=AF.Square, accum_out=s_[:, 0:1]),
                 reads=[lk], writes=['junk', sk])
            rstd_from_ss(C, s_[:, 0:1], 1, 1024, sk, sk)
            ni = rot(C, 'nb', 2)
            P.op('dve', lambda e: e.scalar_tensor_tensor(out=nb[ni][:, 0:1024], in0=lr_[:, 0:1024], scalar=s_[:, 0:1], in1=gql[:],
                                                         op0=ALU.mult, op1=ALU.mult), reads=[lk, sk, 'gql'], writes=['nb%d' % ni])
            ci_ = rot(C, 'cT', 2)

            def dstq(i, g, src, bkey):
                copy_alt(C, cT[ci_][:, i:i + g, :], src.rearrange("p (a b) -> p a b", b=128), [bkey], ['cT%d' % ci_])
            transpose_to(C, lambda i: nb[ni][:, i * 128:(i + 1) * 128], 8, dstq, ['nb%d' % ni], None)
            for c4 in range(4):
                bi = 2 + rot(C, 'psM', 2)
                ps, pskey = C.pb[bi], 'pb%d' % bi
                for kc in range(8):
                    P.op('pe', lambda e: e.matmul(ps[:, 0:384], lhsT=cT[ci_][:, kc, :], rhs=wuq[:, kc, c4 * 384:(c4 + 1) * 384],
                                                  start=(kc == 0), stop=(kc == 7)), reads=['cT%d' % ci_, 'wuq'], writes=[pskey])
                xi = rot(C, 'xsm', 3)
                x_, xk = xs[xi], 'xsm%d' % xi
                t_, tk = tm[xi], 'tmm%d' % xi
                P.op('act', lambda e: e.activation(out=x_[:, 0:384], in_=ps[:, 0:384], func=AF.Copy), reads=[pskey], writes=[xk])
                x3 = x_[:, 0:384].rearrange("p (h c) -> p h c", c=192)
                si = rot(C, 'ss1', 4)
                s_, sk = ss1[si], 'ss1_%d' % si
                P.op('dve', lambda e: e.tensor_tensor(out=t_[:, 0:384], in0=x_[:, 0:384], in1=x_[:, 0:384], op=ALU.mult), reads=[xk], writes=[tk])
                P.op('dve', lambda e: e.tensor_reduce(out=s_[:, 0:2], in_=t_[:, 0:384].rearrange("p (h c) -> p h c", c=192), axis=AX.X, op=ALU.add),
                     reads=[tk], writes=[sk])
                rstd_from_ss(C, s_[:, 0:2], 2, 192, sk, sk)
                P.op('dve', lambda e: e.tensor_tensor(out=x3, in0=x3, in1=s_[:, 0:2].unsqueeze(2).to_broadcast([128, 2, 192]), op=ALU.mult),
                     reads=[xk, sk], writes=[xk])
                P.op('dve', lambda e: e.tensor_tensor(out=x3, in0=x3, in1=gmq[:].unsqueeze(1).to_broadcast([128, 2, 192]), op=ALU.mult),
                     reads=[xk, 'gmq'], writes=[xk])
                oi = rot(C, 'og', 4)
                o3 = og[oi][:].rearrange("p (h c) -> p h c", c=256)
                ok = 'og%d' % oi
                P.op('act', lambda e: e.activation(out=o3[:, :, 0:128], in_=x3[:, :, 0:128], func=AF.Copy), reads=[xk], writes=[ok])
                rope32(x3[:, :, 128:160], x3[:, :, 160:192], o3[:, :, 128:160], o3[:, :, 160:192], t, [xk], ok, 2)
                P.dma('sp', d['qkv'][rows, QO['qm'] + c4 * 512:QO['qm'] + (c4 + 1) * 512], og[oi][:], reads=[ok], writes=[('qkv', t)])
            si = rot(C, 'ss1', 4)
            s_, sk = ss1[si], 'ss1_%d' % si
            P.op('act', lambda e: e.activation(out=junk[:, 0:512], in_=lr_[:, 1024:1536], func=AF.Square, accum_out=s_[:, 0:1]),
                 reads=[lk], writes=['junk', sk])
            rstd_from_ss(C, s_[:, 0:1], 1, 512, sk, sk)
            ni = rot(C, 'nb', 2)
            P.op('dve', lambda e: e.scalar_tensor_tensor(out=nb[ni][:, 0:512], in0=lr_[:, 1024:1536], scalar=s_[:, 0:1], in1=gkvl[:],
                                                         op0=ALU.mult, op1=ALU.mult), reads=[lk, sk, 'gkvl'], writes=['nb%d' % ni])
            ci_ = rot(C, 'cT', 2)

            def dstk(i, g, src, bkey):
                copy_alt(C, cT[ci_][:, i:i + g, :], src.rearrange("p (a b) -> p a b", b=128), [bkey], ['cT%d' % ci_])
            transpose_to(C, lambda i: nb[ni][:, i * 128:(i + 1) * 128], 4, dstk, ['nb%d' % ni], None)
            spi = rot(C, 'spb', 2)
            sp_, spk = spb[spi], 'spb%d' % spi
            P.op('act', lambda e: e.activation(out=junk[:, 0:64], in_=lr_[:, 1536:1600], func=AF.Square, accum_out=sp_[:, 0:1]),
                 reads=[lk], writes=['junk', spk])
            for c4 in range(4):
                bi = 2 + rot(C, 'psM', 2)
                ps, pskey = C.pb[bi], 'pb%d' % bi
                for kc in range(4):
                    P.op('pe', lambda e: e.matmul(ps[:, 0:512], lhsT=cT[ci_][:, kc, :], rhs=wukv[:, kc, c4 * 512:(c4 + 1) * 512],
                                                  start=(kc == 0), stop=(kc == 3)), reads=['cT%d' % ci_, 'wukv'], writes=[pskey])
                xi = rot(C, 'xsm', 3)
                x_, xk = xs[xi], 'xsm%d' % xi
                t_, tk = tm[xi], 'tmm%d' % xi
                P.op('act', lambda e: e.activation(out=x_[:], in_=ps[:], func=AF.Copy), reads=[pskey], writes=[xk])
                x4 = x_[:].rearrange("p (h s c) -> p h s c", s=2, c=128)
                vi = rot(C, 'ov', 3)
                P.op('act', lambda e: e.activation(out=ov[vi][:].rearrange("p (h c) -> p h c", c=128), in_=x4[:, :, 1, :], func=AF.Copy),
                     reads=[xk], writes=['ov%d' % vi])
                P.dma('sp', d['kv'][rows, KO['vm'] + c4 * 256:KO['vm'] + (c4 + 1) * 256], ov[vi][:], reads=['ov%d' % vi], writes=[('kv', t)])
                si = rot(C, 'ss1', 4)
                s_, sk = ss1[si], 'ss1_%d' % si
                t4 = t_[:].rearrange("p (h s c) -> p h s c", s=2, c=128)
                P.op('dve', lambda e: e.tensor_tensor(out=t4[:, :, 0, :], in0=x4[:, :, 0, :], in1=x4[:, :, 0, :], op=ALU.mult), reads=[xk], writes=[tk])
                P.op('dve', lambda e: e.tensor_reduce(out=s_[:, 0:2], in_=t4[:, :, 0, :], axis=AX.X, op=ALU.add), reads=[tk], writes=[sk])
                P.op('dve', lambda e: e.tensor_scalar(out=s_[:, 0:2], in0=s_[:, 0:2], scalar1=sp_[:, 0:1], scalar2=None, op0=ALU.add),
                     reads=[sk, spk], writes=[sk])
                rstd_from_ss(C, s_[:, 0:2], 2, 192, sk, sk)
                oi = rot(C, 'og', 4)
                o3 = og[oi][:].rearrange("p (h c) -> p h c", c=256)
                ok = 'og%d' % oi
                P.op('dve', lambda e: e.tensor_tensor(out=t4[:, :, 0, :], in0=x4[:, :, 0, :], in1=s_[:, 0:2].unsqueeze(2).to_broadcast([128, 2, 128]), op=ALU.mult),
                     reads=[xk, sk], writes=[tk])
                P.op('dve', lambda e: e.tensor_tensor(out=o3[:, :, 0:128], in0=t4[:, :, 0, :], in1=gmk[:, 0:128].unsqueeze(1).to_broadcast([128, 2, 128]), op=ALU.mult),
                     reads=[tk, 'gmk'], writes=[ok])
                ki = rot(C, 'kpn', 3)
                P.op('dve', lambda e: e.tensor_tensor(out=kpn[ki][:], in0=lr_[:, 1536:1600], in1=gmk[:, 128:192], op=ALU.mult),
                     reads=[lk, 'gmk'], writes=['kpn%d' % ki])
                kpt = tm[(xi + 1) % 3]
                kptk = 'tmm%d' % ((xi + 1) % 3)
                kp3 = kpt[:, 0:128].rearrange("p (h c) -> p h c", c=64)
                P.op('dve', lambda e: e.tensor_tensor(out=kp3, in0=kpn[ki][:].unsqueeze(1).to_broadcast([128, 2, 64]),
                                                      in1=s_[:, 0:2].unsqueeze(2).to_broadcast([128, 2, 64]), op=ALU.mult),
                     reads=['kpn%d' % ki, sk], writes=[kptk])
                rope32(kp3[:, :, 0:32], kp3[:, :, 32:64], o3[:, :, 128:160], o3[:, :, 160:192], t, [kptk], ok, 2)
                P.dma('sp', d['kv'][rows, KO['km'] + c4 * 512:KO['km'] + (c4 + 1) * 512], og[oi][:], reads=[ok], writes=[('kv', t)])
        P.barrier()


def stage_ret(C, l, upd_ctx, heads=range(12)):
    P, d = C.P, C.d
    with ExitStack() as st:
        ti = P.sb(st, "ti32", [128, 128], I32)
        dif = P.sb(st, "dif", [128, 128], F32)
        pos = [P.sb(st, "posd%d" % i, [128, 128], F32) for i in range(2)]
        msk = [P.sb(st, "mskd%d" % i, [128, 128], F32) for i in range(2)]
        rowv = [P.sb(st, "rowv%d" % i, [128, 128], F32) for i in range(2)]
        colv = P.sb(st, "colv", [128, 2], F32)
        lg = P.sb(st, "lg", [128, 2, 12], F32)
        cd = P.sb(st, "cd", [128, 2, 12], F32)
        kd = P.sb(st, "kd", [128, 2, 12], F32)
        P.op('pool', lambda e: e.iota(out=ti[:], pattern=[[1, 128]], base=0, channel_multiplier=-1), writes=['ti32'])
        P.op('dve', lambda e: e.tensor_copy(out=dif[:], in_=ti[:]), reads=['ti32'], writes=['dif'])
        P.op('dve', lambda e: e.tensor_scalar(out=pos[0][:], in0=dif[:], scalar1=0.0, scalar2=None, op0=ALU.max), reads=['dif'], writes=['posd0'])
        P.op('dve', lambda e: e.tensor_scalar(out=pos[1][:], in0=dif[:], scalar1=-1.0, scalar2=0.0, op0=ALU.mult, op1=ALU.max), reads=['dif'], writes=['posd1'])
        P.op('dve', lambda e: e.tensor_scalar(out=msk[0][:], in0=dif[:], scalar1=0.0, scalar2=None, op0=ALU.is_ge), reads=['dif'], writes=['mskd0'])
        P.op('dve', lambda e: e.tensor_scalar(out=msk[1][:], in0=dif[:], scalar1=0.0, scalar2=None, op0=ALU.is_le), reads=['dif'], writes=['mskd1'])
        P.op('pool', lambda e: e.iota(out=ti[:], pattern=[[1, 128]], base=1, channel_multiplier=0), reads=['ti32'], writes=['ti32'])
        P.op('dve', lambda e: e.tensor_copy(out=rowv[0][:], in_=ti[:]), reads=['ti32'], writes=['rowv0'])
        P.op('pool', lambda e: e.iota(out=ti[:], pattern=[[-1, 128]], base=128, channel_multiplier=0), reads=['ti32'], writes=['ti32'])
        P.op('dve', lambda e: e.tensor_copy(out=rowv[1][:], in_=ti[:]), reads=['ti32'], writes=['rowv1'])
        P.op('pool', lambda e: e.iota(out=ti[:, 0:1], pattern=[[1, 1]], base=127, channel_multiplier=-1), reads=['ti32'], writes=['ti32'])
        P.op('dve', lambda e: e.tensor_copy(out=colv[:, 0:1], in_=ti[:, 0:1]), reads=['ti32'], writes=['colv'])
        P.op('pool', lambda e: e.iota(out=ti[:, 0:1], pattern=[[1, 1]], base=0, channel_multiplier=1), reads=['ti32'], writes=['ti32'])
        P.op('dve', lambda e: e.tensor_copy(out=colv[:, 1:2], in_=ti[:, 0:1]), reads=['ti32'], writes=['colv'])
        bcast_load(C, lg[:, 0, :], d['ret_decay_f'][l, :], 'lg')
        bcast_load(C, lg[:, 1, :], d['ret_decay_b'][l, :], 'lg')
        P.op('act', lambda e: e.activation(out=lg[:], in_=lg[:], func=AF.Exp), reads=['lg'], writes=['lg'])
        P.op('dve', lambda e: e.tensor_scalar(out=lg[:], in0=lg[:], scalar1=-1.0, scalar2=None, op0=ALU.mult), reads=['lg'], writes=['lg'])
        P.op('act', lambda e: e.activation(out=cd[:], in_=lg[:], func=AF.Exp, scale=128.0), reads=['lg'], writes=['cd'])
        for dr in range(2):
            P.op('dve', lambda e: e.tensor_scalar(out=kd[:, dr, :], in0=lg[:, dr, :], scalar1=colv[:, dr:dr + 1], scalar2=None, op0=ALU.mult),
                 reads=['lg', 'colv'], writes=['kd'])
        P.op('act', lambda e: e.activation(out=kd[:], in_=kd[:], func=AF.Exp), reads=['kd'], writes=['kd'])

        NS = 2
        B = []
        for sl in range(NS):
            b = Ctx()
            b.KT = P.sb(st, "rKT%d" % sl, [128, ROWS], BF16)
            b.QT = P.sb(st, "rQT%d" % sl, [128, ROWS], BF16)
            b.Qd = P.sb(st, "rQd%d" % sl, [128, NTG, 128], BF16)
            b.Kt = P.sb(st, "rKt%d" % sl, [128, NTG, 128], BF16)
            b.Kd = P.sb(st, "rKd%d" % sl, [128, NTG, 128], BF16)
            b.V = P.sb(st, "rV%d" % sl, [128, NTG, 128], BF16)
            b.G = P.sb(st, "rG%d" % sl, [128, NTG, 128], BF16)
            b.yacc = P.sb(st, "ryacc%d" % sl, [128, NTG, 128], F32)
            b.DT = P.sb(st, "rDT%d" % sl, [128, 128], F32)
            b.qd = P.sb(st, "rqd%d" % sl, [128, 128], F32)
            b.S32 = P.sb(st, "rS32_%d" % sl, [128, 128], F32)
            b.S16 = P.sb(st, "rS16_%d" % sl, [128, 128], BF16)
            b.sT = [P.sb(st, "rsT%d_%d" % (sl, i), [128, 128], BF16) for i in range(2)]
            b.st6 = [P.sb(st, "rst6_%d_%d" % (sl, i), [128, 6], F32) for i in range(2)]
            b.mv = [P.sb(st, "rmv%d_%d" % (sl, i), [128, 2], F32) for i in range(2)]
            b.on = [P.sb(st, "ron%d_%d" % (sl, i), [128, 128], F32) for i in range(2)]
            b.yb = [P.sb(st, "ryb%d_%d" % (sl, i), [128, 128], BF16) for i in range(2)]
            b.yT = [P.sb(st, "ryT%d_%d" % (sl, i), [128, 128], BF16) for i in range(2)]
            b.sl = sl
            b.ps_s, b.ps_o, b.ps_u = C.pb[3 * sl], C.pb[3 * sl + 1], C.pb[3 * sl + 2]
            b.k = lambda n, b=b: "r%s%d" % (n, b.sl)
            B.append(b)
        row_chunks = [(0, 1024), (1024, 2048), (2048, 3072), (3072, 4096), (4096, ROWS)]
        hl = list(heads)
        for hp in range(0, len(hl), NS):
            grp = hl[hp:hp + NS]
            for b, h in zip(B, grp):
                b.h = h
                ko, vo, qo = KO['kr'] + h * 128, KO['vr'] + h * 128, QO['qr'] + h * 128
                for (r0, r1) in row_chunks:
                    P.dma('sp', b.KT[:, r0:r1], d['kv'][r0:r1, ko:ko + 128], reads=[('kv', t) for t in range(NTG)], writes=[b.k('KT')], transpose=True)
                    P.dma('sp', b.QT[:, r0:r1], d['qkv'][r0:r1, qo:qo + 128], reads=[('qkv', t) for t in range(NTG)], writes=[b.k('QT')], transpose=True)
                P.dma('sp', b.Kt[:], d['kv'][:, ko:ko + 128].rearrange("(t p) c -> p t c", p=128), reads=[('kv', t) for t in range(NTG)], writes=[b.k('Kt')])
                P.dma('sp', b.V[:], d['kv'][:, vo:vo + 128].rearrange("(t p) c -> p t c", p=128), reads=[('kv', t) for t in range(NTG)], writes=[b.k('V')])
            for dr in range(2):
                order = list(range(NTG)) if dr == 0 else [1, 0] + list(range(NTG - 1, 1, -1))
                for b in B[:len(grp)]:
                    h = b.h
                    go = (QO['gf'] if dr == 0 else QO['gb']) + h * 128
                    P.dma('sp', b.G[:], d['qkv'][:, go:go + 128].rearrange("(t p) c -> p t c", p=128), reads=[('qkv', t) for t in range(NTG)], writes=[b.k('G')])
                    P.op('act', lambda e: e.activation(out=b.DT[:], in_=pos[dr][:], func=AF.Exp, scale=lg[:, dr, h:h + 1]),
                         reads=['posd%d' % dr, 'lg'], writes=[b.k('DT')])
                    P.op('dve', lambda e: e.tensor_tensor(out=b.DT[:], in0=b.DT[:], in1=msk[dr][:], op=ALU.mult), reads=[b.k('DT'), 'mskd%d' % dr], writes=[b.k('DT')])
                    P.op('act', lambda e: e.activation(out=b.qd[:], in_=rowv[dr][:], func=AF.Exp, scale=lg[:, dr, h:h + 1]),
                         reads=['rowv%d' % dr, 'lg'], writes=[b.k('qd')])
                    P.op('dve', lambda e: e.tensor_tensor(out=b.Qd[:], in0=b.QT[:].rearrange("p (t c) -> p t c", c=128),
                                                          in1=b.qd[:].unsqueeze(1).to_broadcast([128, NTG, 128]), op=ALU.mult),
                         reads=[b.k('QT'), b.k('qd')], writes=[b.k('Qd')])
                    P.op('pool', lambda e: e.tensor_scalar(out=b.Kd[:], in0=b.Kt[:], scalar1=kd[:, dr, h:h + 1], scalar2=None, op0=ALU.mult),
                         reads=[b.k('Kt'), 'kd'], writes=[b.k('Kd')])
                    P.op('pool', lambda e: e.memset(b.S32[:], 0.0), writes=[b.k('S32')])
                    P.op('pool', lambda e: e.memset(b.S16[:], 0.0), writes=[b.k('S16')])
                for n, c in enumerate(order):
                    need_out = (c >= 2) or upd_ctx
                    cs = slice(c * 128, (c + 1) * 128)
                    for b in B[:len(grp)]:
                        h = b.h
                        if need_out:
                            i2 = n % 2
                            P.op('pe', lambda e: e.matmul(b.ps_s[:, 0:128], lhsT=b.KT[:, cs], rhs=b.QT[:, cs], start=True, stop=True),
                                 reads=[b.k('KT'), b.k('QT')], writes=[b.k('ps_s')])
                            P.op('dve', lambda e: e.tensor_tensor(out=b.sT[i2][:], in0=b.ps_s[:, 0:128], in1=b.DT[:], op=ALU.mult),
                                 reads=[b.k('ps_s'), b.k('DT')], writes=[b.k('sT%d' % i2)])
                            P.op('pe', lambda e: e.matmul(b.ps_o[:, 0:128], lhsT=b.sT[i2][:], rhs=b.V[:, c, :], start=True, stop=False),
                                 reads=[b.k('sT%d' % i2), b.k('V')], writes=[b.k('ps_o')])
                            P.op('pe', lambda e: e.matmul(b.ps_o[:, 0:128], lhsT=b.Qd[:, c, :], rhs=b.S16[:], start=False, stop=True),
                                 reads=[b.k('Qd'), b.k('S16')], writes=[b.k('ps_o')])
                            P.op('dve', lambda e: e.bn_stats(out=b.st6[i2][:], in_=b.ps_o[:, 0:128]), reads=[b.k('ps_o')], writes=[b.k('st6%d' % i2)])
                            P.op('dve', lambda e: e.bn_aggr(out=b.mv[i2][:], in_=b.st6[i2][:]), reads=[b.k('st6%d' % i2)], writes=[b.k('mv%d' % i2)])
                            P.op('dve', lambda e: e.tensor_scalar(out=b.mv[i2][:, 1:2], in0=b.mv[i2][:, 1:2], scalar1=EPS, scalar2=None, op0=ALU.add),
                                 reads=[b.k('mv%d' % i2)], writes=[b.k('mv%d' % i2)])
                            P.op('pool', lambda e: e.tensor_tensor(out=b.mv[i2][:, 1:2], in0=b.mv[i2][:, 1:2], in1=C.neghalf[:, 0:1], op=ALU.pow),
                                 reads=[b.k('mv%d' % i2), 'neghalf'], writes=[b.k('mv%d' % i2)])
                            P.op('dve', lambda e: e.tensor_scalar(out=b.on[i2][:], in0=b.ps_o[:, 0:128], scalar1=b.mv[i2][:, 0:1], scalar2=b.mv[i2][:, 1:2],
                                                                  op0=ALU.subtract, op1=ALU.mult),
                                 reads=[b.k('ps_o'), b.k('mv%d' % i2)], writes=[b.k('on%d' % i2)])
                            if dr == 0:
                                P.op('pool', lambda e: e.tensor_tensor(out=b.yacc[:, c, :], in0=b.on[i2][:], in1=b.G[:, c, :], op=ALU.mult),
                                     reads=[b.k('on%d' % i2), b.k('G')], writes=[b.k('yacc')])
                            else:
                                P.op('pool', lambda e: e.tensor_tensor(out=b.on[i2][:], in0=b.on[i2][:], in1=b.G[:, c, :], op=ALU.mult),
                                     reads=[b.k('on%d' % i2), b.k('G')], writes=[b.k('on%d' % i2)])
                                P.op('pool', lambda e: e.tensor_tensor(out=b.yb[i2][:], in0=b.on[i2][:], in1=b.yacc[:, c, :], op=ALU.add),
                                     reads=[b.k('on%d' % i2), b.k('yacc')], writes=[b.k('yb%d' % i2)])
                                bi = rot(C, 'pt', 2)
                                P.op('pe', lambda e: e.transpose(out=C.pt[bi][:, 0:128], in_=b.yb[i2][:], identity=C.ident[:]),
                                     reads=[b.k('yb%d' % i2), 'ident'], writes=['pt%d' % bi])
                                P.op('act', lambda e: e.activation(out=b.yT[i2][:], in_=C.pt[bi][:, 0:128], func=AF.Copy),
                                     reads=['pt%d' % bi], writes=[b.k('yT%d' % i2)])
                                P.dma('sp', d['catT'][(20 + h) * 128:(21 + h) * 128, cs], b.yT[i2][:], reads=[b.k('yT%d' % i2)], writes=[('catT', c)])
                        if n < len(order) - 1:
                            P.op('pe', lambda e: e.matmul(b.ps_u[:, 0:128], lhsT=b.Kd[:, c, :], rhs=b.V[:, c, :], start=True, stop=True),
                                 reads=[b.k('Kd'), b.k('V')], writes=[b.k('ps_u')])
                            P.op('dve', lambda e: e.scalar_tensor_tensor(out=b.S32[:], in0=b.S32[:], scalar=cd[:, dr, h:h + 1], in1=b.ps_u[:, 0:128],
                                                                         op0=ALU.mult, op1=ALU.add),
                                 reads=[b.k('S32'), b.k('ps_u'), 'cd'], writes=[b.k('S32')])
                            P.op('act', lambda e: e.activation(out=b.S16[:], in_=b.S32[:], func=AF.Copy), reads=[b.k('S32')], writes=[b.k('S16')])
        P.barrier()


def stage_attn(C, l, tiles, upd_ctx, heads=range(20)):
    P, d = C.P, C.d
    g0 = C.g0
    with ExitStack() as st:
        KT = [P.sb(st, "aKT%d" % i, [128, ROWS], BF16) for i in range(2)]
        KT2 = [P.sb(st, "aKT2_%d" % i, [128, ROWS], BF16) for i in range(2)]
        V = [P.sb(st, "aV%d" % i, [128, NTG, 128], BF16) for i in range(2)]
        QT = [P.sb(st, "aQT%d" % i, [128, TT], BF16) for i in range(2)]
        QT2 = [P.sb(st, "aQT2_%d" % i, [128, TT], BF16) for i in range(2)]
        PT = [P.sb(st, "aPT%d" % i, [128, 512], BF16) for i in range(3)]
        rec = [P.sb(st, "arec%d" % i, [128, 512], F32) for i in range(2)]
        row_chunks = [(0, 1024), (1024, 2048), (2048, 3072), (3072, 4096), (4096, ROWS)]
        allkv = [('kv', t) for t in range(NTG)]
        blocks = []
        lat = [t for t in tiles if t >= 2]
        if upd_ctx and any(t < 2 for t in tiles):
            blocks.append((0, 256, [0, 1]))
        for i in range(0, len(lat), 4):
            tt = lat[i:i + 4]
            blocks.append(((tt[0] - g0) * 128, len(tt) * 128, list(range(NTG))))
        ng = tiles[-1] + 1 - g0
        for hi in heads:
            if hi < 8:
                ko, vo, qo, scale, mla = KO['km'] + hi * 256, KO['vm'] + hi * 128, QO['qm'] + hi * 256, 192.0 ** -0.5, True
            else:
                hq = hi - 8
                ko, vo, qo, scale, mla = KO['kg'] + (hq // 3) * 128, KO['vg'] + (hq // 3) * 128, QO['qg'] + hq * 128, 128.0 ** -0.5, False
            si = rot(C, 'aset', 2)
            kT, kT2, v_, qT, qT2 = KT[si], KT2[si], V[si], QT[si], QT2[si]
            kk = lambda n: "a%s%d" % (n, si)
            for (r0, r1) in row_chunks:
                P.dma('sp', kT[:, r0:r1], d['kv'][r0:r1, ko:ko + 128], reads=allkv, writes=[kk('KT')], transpose=True)
                if mla:
                    P.dma('sp', kT2[:, r0:r1], d['kv'][r0:r1, ko + 128:ko + 256], reads=allkv, writes=[kk('KT2')], transpose=True)
            P.dma('sp', v_[:], d['kv'][:, vo:vo + 128].rearrange("(t p) c -> p t c", p=128), reads=allkv, writes=[kk('V')])
            qr0, qr1 = g0 * 128, (g0 + ng) * 128
            P.dma('sp', qT[:, 0:ng * 128], d['qkv'][qr0:qr1, qo:qo + 128], reads=[('qkv', t) for t in range(NTG)], writes=[kk('QT')], transpose=True)
            if mla:
                P.dma('sp', qT2[:, 0:ng * 128], d['qkv'][qr0:qr1, qo + 128:qo + 256], reads=[('qkv', t) for t in range(NTG)], writes=[kk('QT2')], transpose=True)
            for (q0, nq, kts) in blocks:
                oi = rot(C, 'aO', 2)
                psO, psD = C.pb[2 + oi], C.pb[4 + oi]
                kO, kD = 'pb%d' % (2 + oi), 'pb%d' % (4 + oi)

                def emitS(i):
                    kt = kts[i]
                    ps = C.pb[i % 2]
                    P.op('pe', lambda e: e.matmul(ps[:, 0:nq], lhsT=kT[:, kt * 128:(kt + 1) * 128], rhs=qT[:, q0:q0 + nq], start=True, stop=not mla),
                         reads=[kk('KT'), kk('QT')], writes=['pb%d' % (i % 2)])
                    if mla:
                        P.op('pe', lambda e: e.matmul(ps[:, 0:nq], lhsT=kT2[:, kt * 128:(kt + 1) * 128], rhs=qT2[:, q0:q0 + nq], start=False, stop=True),
                             reads=[kk('KT2'), kk('QT2')], writes=['pb%d' % (i % 2)])
                emitS(0)
                nk = len(kts)
                for i, kt in enumerate(kts):
                    if i + 1 < nk:
                        emitS(i + 1)
                    pi = rot(C, 'aPT', 3)
                    P.op('act', lambda e: e.activation(out=PT[pi][:, 0:nq], in_=C.pb[i % 2][:, 0:nq], func=AF.Exp, scale=scale),
                         reads=['pb%d' % (i % 2)], writes=['aPT%d' % pi])
                    P.op('pe', lambda e: e.matmul(psO[:, 0:nq], lhsT=v_[:, kt, :], rhs=PT[pi][:, 0:nq], start=(i == 0), stop=(i == nk - 1)),
                         reads=[kk('V'), 'aPT%d' % pi], writes=[kO])
                    P.op('pe', lambda e: e.matmul(psD[:, 0:nq], lhsT=C.ones[:], rhs=PT[pi][:, 0:nq], start=(i == 0), stop=(i == nk - 1)),
                         reads=['ones', 'aPT%d' % pi], writes=[kD])
                ri = rot(C, 'arec', 2)
                P.op('dve', lambda e: e.reciprocal(out=rec[ri][:, 0:nq], in_=psD[:, 0:nq]), reads=[kD], writes=['arec%d' % ri])
                P.op('dve', lambda e: e.tensor_tensor(out=C.big3[:, hi, q0:q0 + nq], in0=psO[:, 0:nq], in1=rec[ri][:, 0:nq], op=ALU.mult),
                     reads=[kO, 'arec%d' % ri], writes=[('cat', hi)])
        P.barrier()


def stage_wout(C, l, tiles, xsrc):
    P, d = C.P, C.d
    g0 = C.g0
    with ExitStack() as st:
        C.wbuf = [P.sb(st, "wbuf%d" % i, [128, 32, 512], BF16) for i in range(2)]
        GA = [P.sb(st, "GA%d" % r, [128, D], F32) for r in range(2)]
        xt = [P.sb(st, "xo%d" % i, [128, 512], F32) for i in range(3)]
        tmp = [P.sb(st, "xtmp%d" % i, [128, 512], F32) for i in range(3)]
        rows_needed = [0, 1] if any(t < 2 for t in tiles) else [0]
        for r in rows_needed:
            mod_bcast(C, GA[r][:], l, r, 2 * D, D, 'GA%d' % r)
        t0, t1 = tiles[0], tiles[-1] + 1
        P.dma('sp', C.big3[:, 20:32, (t0 - g0) * 128:(t1 - g0) * 128],
              d['catT'][20 * 128:32 * 128, t0 * 128:t1 * 128].rearrange("(k p) t -> p k t", p=128),
              reads=[('catT', c) for c in range(NTG)], writes=[('cat', 20)])

        def evac(ci):
            def f(t, ps, pskey):
                r = 1 if t < 2 else 0
                rows, cols = slice(t * 128, (t + 1) * 128), slice(ci * 512, (ci + 1) * 512)
                xi = rot(C, 'xo', 3)
                P.dma('sp', xt[xi][:], xsrc[rows, cols], reads=[('x', t)], writes=['xo%d' % xi])
                P.op('dve', lambda e: e.tensor_tensor(out=tmp[xi][:], in0=ps[:], in1=GA[r][:, cols], op=ALU.mult),
                     reads=[pskey, 'GA%d' % r], writes=['xtmp%d' % xi])
                P.op('pool', lambda e: e.tensor_tensor(out=tmp[xi][:], in0=tmp[xi][:], in1=xt[xi][:], op=ALU.add),
                     reads=['xtmp%d' % xi, 'xo%d' % xi], writes=['xtmp%d' % xi])
                P.dma('sp', d['x1'][rows, cols], tmp[xi][:], reads=['xtmp%d' % xi], writes=[('x1', t)])
            return f
        items = [dict(W=d['w_out'][l], n0=ci * 512, nw=512, KC=32,
                      lhsT=lambda kc, t: C.big3[:, kc, (t - g0) * 128:(t - g0 + 1) * 128],
                      tiles=list(tiles), rkeys=lambda t: [('cat', h) for h in range(21)], evac=evac(ci)) for ci in range(8)]
        run_items(C, items)
        P.barrier()


def alloc_routing(C, st):
    P = C.P
    R = Ctx()
    R.posm = P.sb(st, "Rposm", [128, NTG, NE], F32)
    R.i1 = P.sb(st, "Ri1", [128, NTG], I32)
    R.i2 = P.sb(st, "Ri2", [128, NTG], I32)
    R.w1 = P.sb(st, "Rw1", [128, NTG], F32)
    R.w2 = P.sb(st, "Rw2", [128, NTG], F32)
    R.st_scores = ExitStack()
    R.scores = P.sb(R.st_scores, "Rscores", [128, NTG, NE], F32)
    P.op('pool', lambda e: e.memset(R.scores[:], 0.0), writes=['scores'])
    return R


def stage_route(C, l, tiles, R):
    P, d = C.P, C.d
    lts = list(tiles)
    scores, posm = R.scores, R.posm
    with ExitStack() as st:
        wgt = P.sb(st, "Rwgt", [128, NTG, NE], F32)
        oh1 = P.sb(st, "Roh1", [128, NTG, NE], F32)
        oh2 = P.sb(st, "Roh2", [128, NTG, NE], F32)
        sel = P.sb(st, "rsel", [128, NTG, NE], F32)
        tA = P.sb(st, "rtA", [128, NTG, NE], F32)
        tB = P.sb(st, "rtB", [128, NTG, NE], F32)
        mask = P.sb(st, "rmask", [128, NTG, NE], F32)
        maskb = P.sb(st, "rmaskb", [128, NTG, NE], BF16)
        gs = P.sb(st, "rgs", [128, NTG * 4], F32)
        g1 = P.sb(st, "rg1", [128, NTG * 4], F32)
        mx = P.sb(st, "rmx", [128, NTG], F32)
        brb = P.sb(st, "brb", [128, NE], F32)
        tri = P.sb(st, "tri", [128, 128], BF16)
        trif = P.sb(st, "trif", [128, 128], F32)
        offs = P.sb(st, "roffs", [128, NTG, NE], F32)
        ie32 = P.sb(st, "ie32", [128, NE], I32)
        ief = P.sb(st, "ief", [128, NE], F32)
        bcast_load(C, brb[:], d['b_router'], 'brb')
        P.op('pool', lambda e: e.memset(trif[:], 1.0), writes=['trif'])
        P.op('pool', lambda e: e.affine_select(out=trif[:], in_=trif[:], pattern=[[1, 128]], compare_op=ALU.is_gt, fill=0.0, base=0, channel_multiplier=-1),
             reads=['trif'], writes=['trif'])
        P.op('dve', lambda e: e.tensor_copy(out=tri[:], in_=trif[:]), reads=['trif'], writes=['tri'])
        P.op('pool', lambda e: e.iota(out=ie32[:], pattern=[[CAP, NE]], base=0, channel_multiplier=0), writes=['ie32'])
        P.op('dve', lambda e: e.tensor_copy(out=ief[:], in_=ie32[:]), reads=['ie32'], writes=['ief'])

        def dv(fn, reads, writes):
            P.op('dve', fn, reads=reads, writes=writes)
        dv(lambda e: e.tensor_tensor(out=sel[:], in0=scores[:], in1=brb[:].unsqueeze(1).to_broadcast([128, NTG, NE]), op=ALU.add), ['scores', 'brb'], ['rsel'])
        s4 = sel[:].rearrange("p t (g x) -> p (t g) x", x=4)
        pairs = [(0, 1), (0, 2), (0, 3), (1, 2), (1, 3), (2, 3)]
        for pi, (a_, bb) in enumerate(pairs):
            tgt = gs if pi == 0 else g1
            dv(lambda e: e.tensor_tensor(out=tgt[:], in0=s4[:, :, a_], in1=s4[:, :, bb], op=ALU.add), ['rsel'], ['rgs' if pi == 0 else 'rg1'])
            if pi > 0:
                dv(lambda e: e.tensor_tensor(out=gs[:], in0=gs[:], in1=g1[:], op=ALU.max), ['rgs', 'rg1'], ['rgs'])
        gs3 = gs[:].rearrange("p (t g) -> p t g", g=4)
        dv(lambda e: e.tensor_reduce(out=mx[:], in_=gs3, axis=AX.X, op=ALU.max), ['rgs'], ['rmx'])
        g13 = g1[:].rearrange("p (t g) -> p t g", g=4)
        dv(lambda e: e.tensor_tensor(out=g13, in0=gs3, in1=mx[:].unsqueeze(2).to_broadcast([128, NTG, 4]), op=ALU.is_equal), ['rgs', 'rmx'], ['rg1'])
        dv(lambda e: e.tensor_scalar(out=g1[:], in0=g1[:], scalar1=1e4, scalar2=-1e4, op0=ALU.mult, op1=ALU.add), ['rg1'], ['rg1'])
        tA4 = tA[:].rearrange("p t (g x) -> p (t g) x", x=4)
        dv(lambda e: e.tensor_tensor(out=tA4, in0=s4, in1=g1[:].unsqueeze(2).to_broadcast([128, NTG * 4, 4]), op=ALU.add), ['rsel', 'rg1'], ['rtA'])
        dv(lambda e: e.tensor_reduce(out=mx[:], in_=tA[:], axis=AX.X, op=ALU.max), ['rtA'], ['rmx'])
        dv(lambda e: e.tensor_tensor(out=oh1[:], in0=tA[:], in1=mx[:].unsqueeze(2).to_broadcast([128, NTG, NE]), op=ALU.is_equal), ['rtA', 'rmx'], ['roh1'])
        dv(lambda e: e.scalar_tensor_tensor(out=tB[:], in0=oh1[:], scalar=-1e4, in1=tA[:], op0=ALU.mult, op1=ALU.add), ['roh1', 'rtA'], ['rtB'])
        dv(lambda e: e.tensor_reduce(out=mx[:], in_=tB[:], axis=AX.X, op=ALU.max), ['rtB'], ['rmx'])
        dv(lambda e: e.tensor_tensor(out=oh2[:], in0=tB[:], in1=mx[:].unsqueeze(2).to_broadcast([128, NTG, NE]), op=ALU.is_equal), ['rtB', 'rmx'], ['roh2'])
        for lt_ in range(NTG):
            if lt_ not in lts:
                P.op('pool', lambda e: e.memset(oh1[:, lt_, :], 0.0), reads=['roh1'], writes=['roh1'])
                P.op('pool', lambda e: e.memset(oh2[:, lt_, :], 0.0), reads=['roh2'], writes=['roh2'])
        dv(lambda e: e.tensor_tensor(out=mask[:], in0=oh1[:], in1=oh2[:], op=ALU.add), ['roh1', 'roh2'], ['rmask'])
        dv(lambda e: e.tensor_tensor(out=wgt[:], in0=scores[:], in1=mask[:], op=ALU.mult), ['scores', 'rmask'], ['rwgt'])
        dv(lambda e: e.tensor_reduce(out=mx[:], in_=wgt[:], axis=AX.X, op=ALU.add), ['rwgt'], ['rmx'])
        dv(lambda e: e.tensor_scalar(out=mx[:], in0=mx[:], scalar1=1e-30, scalar2=None, op0=ALU.max), ['rmx'], ['rmx'])
        dv(lambda e: e.reciprocal(out=mx[:], in_=mx[:]), ['rmx'], ['rmx'])
        dv(lambda e: e.tensor_tensor(out=wgt[:], in0=wgt[:], in1=mx[:].unsqueeze(2).to_broadcast([128, NTG, NE]), op=ALU.mult), ['rwgt', 'rmx'], ['rwgt'])
        dv(lambda e: e.tensor_copy(out=maskb[:], in_=mask[:]), ['rmask'], ['rmaskb'])
        mb2 = maskb[:].rearrange("p t e -> p (t e)")
        HW = NTG * NE // 2
        posf = posm[:].rearrange("p t e -> p (t e)")
        tBf = tB[:].rearrange("p t e -> p (t e)")
        for hh in range(2):
            psP, psC = C.pb[2 + 2 * hh], C.pb[3 + 2 * hh]
            cs_ = slice(hh * HW, (hh + 1) * HW)
            P.op('pe', lambda e: e.matmul(psP[:, 0:HW], lhsT=tri[:], rhs=mb2[:, cs_], start=True, stop=True), reads=['tri', 'rmaskb'], writes=['pb%d' % (2 + 2 * hh)])
            P.op('pe', lambda e: e.matmul(psC[:, 0:HW], lhsT=C.ones[:], rhs=mb2[:, cs_], start=True, stop=True), reads=['ones', 'rmaskb'], writes=['pb%d' % (3 + 2 * hh)])
            dv(lambda e: e.tensor_copy(out=tBf[:, cs_], in_=psC[:, 0:HW]), ['pb%d' % (3 + 2 * hh)], ['rtB'])
            dv(lambda e: e.tensor_copy(out=posf[:, cs_], in_=psP[:, 0:HW]), ['pb%d' % (2 + 2 * hh)], ['rposm'])
        P.op('pool', lambda e: e.memset(offs[:], 0.0), writes=['roffs'])
        for k in range(1, NTG):
            dv(lambda e: e.tensor_tensor(out=offs[:, k, :], in0=offs[:, k - 1, :], in1=tB[:, k - 1, :], op=ALU.add), ['roffs', 'rtB'], ['roffs'])
        dv(lambda e: e.tensor_tensor(out=posm[:], in0=posm[:], in1=offs[:], op=ALU.add), ['rposm', 'roffs'], ['rposm'])
        dv(lambda e: e.tensor_tensor(out=posm[:], in0=posm[:], in1=mask[:], op=ALU.mult), ['rposm', 'rmask'], ['rposm'])
        dv(lambda e: e.tensor_tensor(out=posm[:], in0=posm[:], in1=mask[:], op=ALU.add), ['rposm', 'rmask'], ['rposm'])
        dv(lambda e: e.tensor_scalar(out=posm[:], in0=posm[:], scalar1=-1.0, scalar2=None, op0=ALU.add), ['rposm'], ['rposm'])
        dv(lambda e: e.tensor_scalar(out=tB[:], in0=posm[:], scalar1=float(CAP), scalar2=None, op0=ALU.is_lt), ['rposm'], ['rtB'])
        dv(lambda e: e.tensor_tensor(out=wgt[:], in0=wgt[:], in1=tB[:], op=ALU.mult), ['rwgt', 'rtB'], ['rwgt'])
        dv(lambda e: e.tensor_tensor(out=tA[:], in0=posm[:], in1=ief[:].unsqueeze(1).to_broadcast([128, NTG, NE]), op=ALU.add), ['rposm', 'ief'], ['rtA'])
        for (oh, ohk, it_, itk, w_, wk) in ((oh1, 'roh1', R.i1, 'Ri1', R.w1, 'Rw1'), (oh2, 'roh2', R.i2, 'Ri2', R.w2, 'Rw2')):
            dv(lambda e: e.tensor_tensor(out=tB[:], in0=tA[:], in1=oh[:], op=ALU.mult), ['rtA', ohk], ['rtB'])
            dv(lambda e: e.tensor_reduce(out=mx[:], in_=tB[:], axis=AX.X, op=ALU.add), ['rtB'], ['rmx'])
            dv(lambda e: e.tensor_scalar(out=mx[:], in0=mx[:], scalar1=float(NE * CAP - 1), scalar2=0.0, op0=ALU.min, op1=ALU.max), ['rmx'], ['rmx'])
            dv(lambda e: e.tensor_copy(out=it_[:], in_=mx[:]), ['rmx'], [itk])
            dv(lambda e: e.tensor_tensor(out=tB[:], in0=wgt[:], in1=oh[:], op=ALU.mult), ['rwgt', ohk], ['rtB'])
            dv(lambda e: e.tensor_reduce(out=w_[:], in_=tB[:], axis=AX.X, op=ALU.add), ['rtB'], [wk])
        P.barrier()


def stage_ffn_a(C, l, tiles, R):
    P, d = C.P, C.d
    g0 = C.g0
    stage_norm(C, l, d['x1'], 'g_ffn', 3, 4, tiles, tok_dst=d['h2tok'], router=dict(scores=R.scores))
    with ExitStack() as st:
        C.wbuf = [P.sb(st, "wbuf%d" % i, [128, 32, 512], BF16) for i in range(2)]
        GFc = [[P.sb(st, "GF%d_%d" % (r, i), [128, 512], F32) for i in range(2)] for r in range(2)]
        rows_needed = [0, 1] if any(t < 2 for t in tiles) else [0]
        atok = [P.sb(st, "atok%d" % i, [128, 512], BF16) for i in range(2)]
        xt = [P.sb(st, "fx%d" % i, [128, 512], F32) for i in range(2)]
        tmp = [P.sb(st, "ftmp%d" % i, [128, 512], F32) for i in range(2)]
        sg = P.sb(st, "sgs", [128, NT, 512], BF16)
        aT = P.sb(st, "aTs", [128, 8, TT], BF16)
        C.atok = atok

        def mkx(ci):
            state = {}

            def f(t, ps, pskey):
                if 'gi' not in state:
                    state['gi'] = rot(C, 'GFc', 2)
                    for r_ in rows_needed:
                        mod_bcast(C, GFc[r_][state['gi']][:], l, r_, 5 * D + ci * 512, 512, 'GF%d_%d' % (r_, state['gi']))
                gi = state['gi']
                r = 1 if t < 2 else 0
                rows, cols = slice(t * 128, (t + 1) * 128), slice(ci * 512, (ci + 1) * 512)
                xi = rot(C, 'fx', 2)
                P.dma('sp', xt[xi][:], d['x1'][rows, cols], reads=[('x1', t)], writes=['fx%d' % xi])
                P.op('dve', lambda e: e.tensor_tensor(out=tmp[xi][:], in0=ps[:], in1=GFc[r][gi][:], op=ALU.mult),
                     reads=[pskey, 'GF%d_%d' % (r, gi)], writes=['ftmp%d' % xi])
                P.op('pool', lambda e: e.tensor_tensor(out=tmp[xi][:], in0=tmp[xi][:], in1=xt[xi][:], op=ALU.add),
                     reads=['ftmp%d' % xi, 'fx%d' % xi], writes=['ftmp%d' % xi])
                P.dma('sp', d['x1'][rows, cols], tmp[xi][:], reads=['ftmp%d' % xi], writes=[('x1', t)])
            return f
        big_l = lambda kc, t: C.big3[:, kc, (t - g0) * 128:(t - g0 + 1) * 128]
        idx_t = lambda t: t - g0
        items = []
        for ci in range(2):
            items.append(dict(W=d['s_gate'][l], n0=ci * 512, nw=512, KC=32, lhsT=big_l, tiles=list(tiles), rkeys=lambda t: [('big', t)],
                              evac=evac_gate(C, sg, idx_t)))
            items.append(dict(W=d['s_up'][l], n0=ci * 512, nw=512, KC=32, lhsT=big_l, tiles=list(tiles), rkeys=lambda t: [('big', t)],
                              evac=evac_up(C, sg, aT, ci, idx_t, 'aTs')))
        for ci in range(8):
            items.append(dict(W=d['s_down'][l], n0=ci * 512, nw=512, KC=8, lhsT=lambda kc, t: aT[:, kc, (t - g0) * 128:(t - g0 + 1) * 128],
                              tiles=list(tiles), rkeys=lambda t: ['aTs'], evac=mkx(ci)))
        run_items(C, items)
        P.barrier()


def evac_gate(C, sgbuf, idx):
    def f(t, ps, pskey):
        C.P.op('act', lambda e: e.activation(out=sgbuf[:, idx(t), 0:512], in_=ps[:], func=AF.Silu), reads=[pskey], writes=['sg'])
    return f


def evac_up(C, sgbuf, aTbuf, ci, idx, akey):
    def f(t, ps, pskey):
        ai = rot(C, 'atok', 2)
        atok = C.atok
        C.P.op('dve', lambda e: e.tensor_tensor(out=atok[ai][:], in0=ps[:], in1=sgbuf[:, idx(t), 0:512], op=ALU.mult),
               reads=[pskey, 'sg'], writes=['atok%d' % ai])

        def dst_(i, g, src, bkey):
            copy_alt(C, aTbuf[:, ci * 4 + i:ci * 4 + i + g, idx(t) * 128:(idx(t) + 1) * 128], src.rearrange("p (a b) -> p a b", b=128), [bkey], [akey])
        transpose_to(C, lambda i: atok[ai][:, i * 128:(i + 1) * 128], 4, dst_, ['atok%d' % ai], None)
    return f


def pool_gather(C, out, src, idx_ap, reads, writes):
    P = C.P
    k = P.di % NDMA
    P.di += 1
    toks = P._deps(reads, writes)
    if P.dcnt[k] > 0:
        toks.append((P.dsem[k], 16 * P.dcnt[k]))
    P._emit_waits('pool', toks)
    ins = C.nc.gpsimd.indirect_dma_start(out=out, out_offset=None, in_=src, in_offset=bass.IndirectOffsetOnAxis(ap=idx_ap, axis=0))
    P.dcnt[k] += 1
    ins.then_inc(P.dsem[k], 16)
    tok = (P.dsem[k], 16 * P.dcnt[k])
    P._commit(tok, reads, writes)
    P.n_ops += 1


def stage_ffn_b(C, l, tiles, R, dst, dst_row0):
    P, d = C.P, C.d
    lts = list(tiles)
    NS = CAP // 128
    posm = R.posm
    with ExitStack() as st:
        C.wbuf = [P.sb(st, "wbuf%d" % i, [128, 32, 512], BF16) for i in range(2)]
        atok = [P.sb(st, "atok%d" % i, [128, 512], BF16) for i in range(2)]
        C.atok = atok
        ystg = [P.sb(st, "ystg%d" % i, [128, 512], BF16) for i in range(1)]
        iof = P.sb(st, "iof", [128, CAP], F32)
        tvi = P.sb(st, "tvi", [128, NTG, 2], I32)
        tv = P.sb(st, "tv", [128, NTG, 2], BF16)
        selb = P.sb(st, "selb", [128, CAP], BF16)
        idT = P.sb(st, "idT", [2, CAP], BF16)
        idf = [P.sb(st, "idf%d" % i, [128, 2], F32) for i in range(2)]
        idi = [P.sb(st, "idi%d" % i, [128, 1], I32) for i in range(NS)]
        xtok = [P.sb(st, "xtok%d" % i, [128, D], BF16) for i in range(2)]
        xsT = P.sb(st, "xsT", [128, 32, CAP], BF16)
        sgE = P.sb(st, "sgE", [128, NS, 512], BF16)
        aTE = P.sb(st, "aTE", [128, 8, CAP], BF16)
        P.op('pool', lambda e: e.iota(out=iof[:], pattern=[[1, CAP]], base=0, channel_multiplier=0, allow_small_or_imprecise_dtypes=True), writes=['iof'])
        P.op('pool', lambda e: e.iota(out=tvi[:].rearrange("p t c -> p (t c)"), pattern=[[1, NTG], [0, 2]], base=0, channel_multiplier=0), writes=['tvi'])
        P.op('dve', lambda e: e.tensor_copy(out=tv[:, :, 0:1], in_=tvi[:, :, 0:1]), reads=['tvi'], writes=['tv'])
        P.op('pool', lambda e: e.iota(out=tvi[:].rearrange("p t c -> p (t c)"), pattern=[[0, NTG * 2]], base=0, channel_multiplier=1), reads=['tvi', 'tv'], writes=['tvi'])
        P.op('dve', lambda e: e.tensor_copy(out=tv[:, :, 1:2], in_=tvi[:, :, 1:2]), reads=['tvi'], writes=['tv'])
        colch = [(c0, min(512, CAP - c0)) for c0 in range(0, CAP, 512)]

        def disp_idx(ex):
            for n_, lt in enumerate(lts):
                P.op('dve', lambda e: e.tensor_scalar(out=selb[:], in0=iof[:], scalar1=posm[:, lt, ex:ex + 1], scalar2=None, op0=ALU.is_equal),
                     reads=['iof', 'rposm'], writes=['selb'])
                for ci_, (c0, cw) in enumerate(colch):
                    P.op('pe', lambda e: e.matmul(C.pb[2 + ci_][0:2, 0:cw], lhsT=tv[:, lt, :], rhs=selb[:, c0:c0 + cw], start=(n_ == 0), stop=(n_ == len(lts) - 1)),
                         reads=['selb', 'tv'], writes=['pb%d' % (2 + ci_)])
            for ci_, (c0, cw) in enumerate(colch):
                P.op('act', lambda e: e.activation(out=idT[:, c0:c0 + cw], in_=C.pb[2 + ci_][0:2, 0:cw], func=AF.Copy), reads=['pb%d' % (2 + ci_)], writes=['idT'])
            for st_ in range(NS):
                bi = rot(C, 'pt', 2)
                P.op('pe', lambda e: e.transpose(out=C.pt[bi][:, 0:2], in_=idT[0:2, st_ * 128:(st_ + 1) * 128], identity=C.ident[0:2, 0:2]),
                     reads=['idT', 'ident'], writes=['pt%d' % bi])
                fi = rot(C, 'idf', 2)
                P.op('dve', lambda e: e.tensor_copy(out=idf[fi][:], in_=C.pt[bi][:, 0:2]), reads=['pt%d' % bi], writes=['idf%d' % fi])
                P.op('dve', lambda e: e.scalar_tensor_tensor(out=idf[fi][:, 0:1], in0=idf[fi][:, 0:1], scalar=128.0, in1=idf[fi][:, 1:2], op0=ALU.mult, op1=ALU.add),
                     reads=['idf%d' % fi], writes=['idf%d' % fi])
                P.op('dve', lambda e: e.tensor_copy(out=idi[st_][:], in_=idf[fi][:, 0:1]), reads=['idf%d' % fi], writes=['idi%d' % st_])

        def disp_gather(st_):
            xi = st_ % 2
            pool_gather(C, xtok[xi][:, :], d['h2tok'][:, :], idi[st_][:, :], ['idi%d' % st_] + [('h2tok', lt) for lt in range(NTG)], ['xtok%d' % xi])

        def disp_tr(st_):
            xi = st_ % 2

            def dst_(i, g, src, bkey):
                copy_alt(C, xsT[:, i:i + g, st_ * 128:(st_ + 1) * 128], src.rearrange("p (a b) -> p a b", b=128), [bkey], ['xsT'])
            transpose_to(C, lambda i: xtok[xi][:, i * 128:(i + 1) * 128], 32, dst_, ['xtok%d' % xi], None)

        def chain(*fns):
            def f():
                for fn in fns:
                    fn()
            return f
        items = []
        for ex in range(NE):
            xl = lambda kc, t: xsT[:, kc, t * 128:(t + 1) * 128]
            ident_t = lambda t: t
            nxt = ex + 1
            for ci in range(2):
                it = dict(W=d['e_gate'][l, ex], n0=ci * 512, nw=512, KC=32, lhsT=xl, tiles=list(range(NS)), rkeys=lambda t: ['xsT'],
                          evac=evac_gate(C, sgE, ident_t))
                if ci == 0:
                    def pre(ex=ex):
                        disp_idx(ex)
                        disp_gather(0)
                        for s_ in range(NS):
                            if s_ + 1 < NS:
                                disp_gather(s_ + 1)
                            disp_tr(s_)
                    it['pre'] = pre
                items.append(it)
                it2 = dict(W=d['e_up'][l, ex], n0=ci * 512, nw=512, KC=32, lhsT=xl, tiles=list(range(NS)), rkeys=lambda t: ['xsT'],
                           evac=evac_up(C, sgE, aTE, ci, ident_t, 'aTE'))
                if ci == 1 and nxt < NE and PIPE_DISPATCH:
                    it2['post'] = (lambda nxt=nxt: disp_idx(nxt))
                items.append(it2)

            def mky(ci, ex=ex):
                def f(t, ps, pskey):
                    yi = 0
                    copy_alt(C, ystg[yi][:], ps[:], [pskey], ['ystg%d' % yi])
                    P.dma('sp', d['yall'][ex * CAP + t * 128:ex * CAP + (t + 1) * 128, ci * 512:(ci + 1) * 512], ystg[yi][:], reads=['ystg%d' % yi], writes=['yall'])
                return f
            for ci in range(8):
                it3 = dict(W=d['e_down'][l, ex], n0=ci * 512, nw=512, KC=8, lhsT=lambda kc, t: aTE[:, kc, t * 128:(t + 1) * 128],
                           tiles=list(range(NS)), rkeys=lambda t: ['aTE'], evac=mky(ci))
                if nxt < NE and PIPE_DISPATCH:
                    if ci == 0:
                        it3['pre'] = (lambda: disp_gather(0))
                    else:
                        it3['pre'] = chain(lambda ci=ci: disp_gather(ci), lambda ci=ci: disp_tr(ci - 1))
                items.append(it3)
        run_items(C, items)
        P.barrier()
    with ExitStack() as st:
        GF = [P.sb(st, "GFf%d" % r, [128, D], F32) for r in range(2)]
        y1 = [P.sb(st, "y1_%d" % i, [128, D], BF16) for i in range(2)]
        y2 = [P.sb(st, "y2_%d" % i, [128, D], BF16) for i in range(2)]
        xa = [P.sb(st, "xa%d" % i, [128, D], F32) for i in range(2)]
        ta = [P.sb(st, "ta%d" % i, [128, D], F32) for i in range(2)]
        rows_needed = [0, 1] if any(t < 2 for t in tiles) else [0]
        for r in rows_needed:
            mod_bcast(C, GF[r][:], l, r, 5 * D, D, 'GFf%d' % r)
        for t in tiles:
            lt = t
            r = 1 if t < 2 else 0
            bi = rot(C, 'cmb', 2)
            pool_gather(C, y1[bi][:, :], d['yall'][:, :], R.i1[:, lt:lt + 1], ['Ri1', 'yall'], ['y1_%d' % bi])
            pool_gather(C, y2[bi][:, :], d['yall'][:, :], R.i2[:, lt:lt + 1], ['Ri2', 'yall'], ['y2_%d' % bi])
            P.dma('sp', xa[bi][:], d['x1'][t * 128:(t + 1) * 128, :], reads=[('x1', t)], writes=['xa%d' % bi])
            P.op('dve', lambda e: e.tensor_scalar(out=ta[bi][:], in0=y1[bi][:], scalar1=R.w1[:, lt:lt + 1], scalar2=None, op0=ALU.mult),
                 reads=['y1_%d' % bi, 'Rw1'], writes=['ta%d' % bi])
            P.op('dve', lambda e: e.scalar_tensor_tensor(out=ta[bi][:], in0=y2[bi][:], scalar=R.w2[:, lt:lt + 1], in1=ta[bi][:], op0=ALU.mult, op1=ALU.add),
                 reads=['y2_%d' % bi, 'Rw2', 'ta%d' % bi], writes=['ta%d' % bi])
            P.op('pool', lambda e: e.tensor_tensor(out=ta[bi][:], in0=ta[bi][:], in1=GF[r][:], op=ALU.mult), reads=['ta%d' % bi, 'GFf%d' % r], writes=['ta%d' % bi])
            P.op('pool', lambda e: e.tensor_tensor(out=ta[bi][:], in0=ta[bi][:], in1=xa[bi][:], op=ALU.add), reads=['ta%d' % bi, 'xa%d' % bi], writes=['ta%d' % bi])
            P.dma('sp', dst[(t - dst_row0) * 128:(t - dst_row0 + 1) * 128, :], ta[bi][:], reads=['ta%d' % bi], writes=[('xout', t)])
        P.barrier()


WEIGHT_SPECS = {
    'g_mix': ([2, D], F32), 'g_ffn': ([2, D], F32), 'w_mod': ([2, D, 6 * D], F32), 'b_mod': ([2, 6 * D], F32),
    'w_in': ([2, D, DIN], F32),
    'mla_q_lora_g': ([2, 1024], F32), 'mla_kv_lora_g': ([2, 512], F32),
    'w_uq': ([2, 1024, 1536], F32), 'w_ukv': ([2, 512, 2048], F32),
    'mla_q_g': ([2, 192], F32), 'mla_k_g': ([2, 192], F32), 'gqa_q_g': ([2, 128], F32), 'gqa_k_g': ([2, 128], F32),
    'ret_decay_f': ([2, 12], F32), 'ret_decay_b': ([2, 12], F32), 'w_out': ([2, D, D], F32),
    'w_router': ([D, NE], F32), 'b_router': ([NE], F32),
    'e_gate': ([2, NE, D, 1024], F32), 'e_up': ([2, NE, D, 1024], F32), 'e_down': ([2, NE, 1024, D], F32),
    's_gate': ([2, D, 1024], F32), 's_up': ([2, D, 1024], F32), 's_down': ([2, 1024, D], F32),
}
OTHER_SPECS = {
    'xin': ([ROWS, D], F32), 'cT': ([128, 32, 2], F32),
    'rope_mla': ([ROWS, 64], F32), 'rope_gqa': ([ROWS, 128], F32), 'rope_ret': ([ROWS, 128], F32),
    'modv': ([2, 2, 6 * D], F32),
    'lr': ([ROWS, 1600], F32), 'qkv': ([ROWS, QW], BF16), 'kv': ([ROWS, KVW], BF16),
    'catT': ([D, ROWS], BF16),
    'x1': ([ROWS, D], F32), 'xres': ([ROWS, D], F32), 'h2tok': ([ROWS, D], BF16), 'yall': ([NE * CAP, D], BF16),
    'y': ([4096, D], F32), 'dbgr': ([128, 3, NT * NE], F32),
}


class DramSet(dict):
    def __init__(self, nc, ext_in, ext_out):
        super().__init__()
        self.nc, self.ext_in, self.ext_out = nc, set(ext_in), set(ext_out)

    def __missing__(self, name):
        spec = WEIGHT_SPECS.get(name) or OTHER_SPECS[name]
        if name in self.ext_in:
            kind = "ExternalInput"
        elif name in self.ext_out:
            kind = "ExternalOutput"
        else:
            kind = "Internal"
        ap = self.nc.dram_tensor(name, list(spec[0]), spec[1], kind=kind).ap()
        self[name] = ap
        return ap


def new_ctx(ext_in, ext_out):
    nc = bass.Bass("TRN2", target_bir_lowering=False)
    C = Ctx()
    C.nc = nc
    C.gst = ExitStack()
    C.P = Prog(nc, C.gst)
    C.d = DramSet(nc, ext_in, ext_out)
    C.rotc = {}
    C.g0 = 0
    emit_consts(C)
    return C


def alloc_big(C, st):
    C.big = C.P.sb(st, "big", [128, 32 * TT], BF16)
    C.big3 = C.big[:].rearrange("p (k t) -> p k t", t=TT)


def phase_A(C, l, groups=GROUPS):
    xsrc = C.d['xin'] if l == 0 else C.d['xres']
    with ExitStack() as st:
        alloc_big(C, st)
        for (g0, g1) in groups:
            C.g0 = g0
            tiles = list(range(g0, g1))
            stage_norm(C, l, xsrc, 'g_mix', 0, 1, tiles)
            stage_win(C, l, tiles)
            stage_mla(C, l, tiles)
    C.g0 = 0


def rope_tables():
    f32 = np.float32
    s = np.arange(4096).astype(f32)
    row, col = np.floor(s / 64).astype(f32), np.mod(s, 64).astype(f32)

    def freqs(dim):
        return (f32(10000.0) ** (-(np.arange(0, dim, 2, dtype=f32)) / f32(dim))).astype(f32)

    def tab(ang_lat, half, ctx_ang=None):
        t = np.zeros((ROWS, 2 * half), f32)
        t[:256, :half] = 1.0
        if ctx_ang is not None:
            t[:256, :half] = np.cos(ctx_ang)
            t[:256, half:] = np.sin(ctx_ang)
        t[256:, :half] = np.cos(ang_lat)
        t[256:, half:] = np.sin(ang_lat)
        return t
    fm, fg, fr = freqs(32), freqs(64), freqs(128)
    am = np.concatenate([row[:, None] * fm, col[:, None] * fm], axis=1).astype(f32)
    ag = np.concatenate([row[:, None] * fg, col[:, None] * fg], axis=1).astype(f32)
    ar = ((f32(256) + s)[:, None] * fr).astype(f32)
    arc = (np.arange(256, dtype=f32)[:, None] * fr).astype(f32)
    return tab(am, 32), tab(ag, 64), tab(ar, 64, arc)


def core_inputs(inputs, b, names):
    m = {}
    for k in names:
        if k in WEIGHT_SPECS:
            m[k] = np.ascontiguousarray(inputs[k])
    if 'xin' in names:
        m['xin'] = np.ascontiguousarray(np.concatenate([inputs['ctx'][b], inputs['x'][b]], axis=0))
    if 'cT' in names:
        cc = np.stack([inputs['c'][b], inputs['c_ctx']], axis=1)
        m['cT'] = np.ascontiguousarray(cc.reshape(32, 128, 2).transpose(1, 0, 2))
    if 'rope_mla' in names:
        m['rope_mla'], m['rope_gqa'], m['rope_ret'] = rope_tables()
    return m


def phase_mix(C, l, upd_ctx, groups=GROUPS, ffn=True):
    xsrc = C.d['xin'] if l == 0 else C.d['xres']
    with ExitStack() as stl:
        R = alloc_routing(C, stl)
        all_tiles = []
        for (g0, g1) in groups:
            C.g0 = g0
            tiles = [t for t in range(g0, g1) if (t >= 2 or upd_ctx)]
            all_tiles += tiles
            with ExitStack() as st:
                alloc_big(C, st)
                stage_attn(C, l, tiles, upd_ctx)
                stage_wout(C, l, tiles, xsrc)
                if ffn:
                    stage_ffn_a(C, l, tiles, R)
        C.g0 = 0
        if not ffn:
            R.st_scores.close()
        if ffn:
            stage_route(C, l, all_tiles, R)
            R.st_scores.close()
            if l == 0:
                stage_ffn_b(C, l, all_tiles, R, C.d['xres'], 0)
            else:
                stage_ffn_b(C, l, all_tiles, R, C.d['y'], 2)


LAYER_WEIGHTS = ['g_mix', 'g_ffn', 'w_in', 'mla_q_lora_g', 'mla_kv_lora_g', 'w_uq', 'w_ukv', 'mla_q_g', 'mla_k_g', 'gqa_q_g', 'gqa_k_g',
                 'ret_decay_f', 'ret_decay_b', 'w_out', 'w_router', 'b_router', 'e_gate', 'e_up', 'e_down', 's_gate', 's_up', 's_down']
FULL_INPUTS = ['xin', 'cT', 'rope_mla', 'rope_gqa', 'rope_ret', 'w_mod', 'b_mod'] + LAYER_WEIGHTS


def build_full():
    C = new_ctx(FULL_INPUTS, ['y'])
    stage_mod(C)
    for l in range(2):
        upd = (l == 0)
        phase_A(C, l)
        stage_ret(C, l, upd)
        phase_mix(C, l, upd)
    C.P.finish()
    C.gst.close()
    return C.nc


def kernel(**inputs):
    inputs = {k: np.asarray(v) for k, v in inputs.items()}
    nc = build_full()
    in_maps = [core_inputs(inputs, b, FULL_INPUTS) for b in range(2)]
    res = run_bass_kernel_spmd(nc, in_maps, core_ids=[0, 1])
    return np.stack([np.asarray(res.results[b]['y'], dtype=np.float32) for b in range(2)], axis=0)
```
